# Optimizing a Trainium2 kernel written in Bass

```python
import jax, jax.numpy as jnp
from jax import lax
import numpy as np

D_MODEL = 1024
BATCH = 2
SEQ = 8192
DEPTH = 4

PLE_DIM = 256
RET_HEADS = 4
RET_HEAD_DIM = 128
SB_HEADS = 4
SB_HEAD_DIM = 128
RET_WIDTH = RET_HEADS * RET_HEAD_DIM
SB_WIDTH = SB_HEADS * SB_HEAD_DIM
EVEN_WIDTH = RET_WIDTH + SB_WIDTH
EVEN_PROJ = 4 * RET_WIDTH + 4 * SB_WIDTH
RET_CHUNK = 128
SB_BLOCK = 128
ROPE_BASE = 10000.0
RWKV_HEAD_DIM = 64
RWKV_HEADS = D_MODEL // RWKV_HEAD_DIM
DECAY_LORA = 64
ICLR_LORA = 64
VRES_LORA = 32
N_EVEN = (DEPTH + 1) // 2
N_ODD = DEPTH // 2
N_VRES = max(N_ODD - 1, 0)
RMS_EPS = 1e-6
GN_EPS = 1e-5
LNX_EPS = 64e-5

kernel_name = "hybrid_retention_stickbreak_rwkv7_ple"


def rmsnorm(x, gain):
    xf = x.astype(jnp.float32)
    y = xf * lax.rsqrt(jnp.mean(xf * xf, axis=-1, keepdims=True) + RMS_EPS)
    return (y * gain).astype(x.dtype)


def head_norm(y, eps):
    yf = y.astype(jnp.float32)
    mu = jnp.mean(yf, axis=-1, keepdims=True)
    var = jnp.mean(jnp.square(yf - mu), axis=-1, keepdims=True)
    out = (yf - mu) * lax.rsqrt(var + eps)
    return out.reshape(*y.shape[:-2], -1)


def rotary(x, pos):
    half = x.shape[-1] // 2
    inv_freq = ROPE_BASE ** (-jnp.arange(half, dtype=jnp.float32) / half)
    ang = pos.astype(jnp.float32)[:, None] * inv_freq[None, :]
    cos = jnp.cos(ang)[None, :, None, :]
    sin = jnp.sin(ang)[None, :, None, :]
    x1 = x[..., :half].astype(jnp.float32)
    x2 = x[..., half:].astype(jnp.float32)
    return jnp.concatenate([x1 * cos - x2 * sin, x1 * sin + x2 * cos], axis=-1).astype(x.dtype)


def retention(q, k, v, gn_gain):
    B, S, H, Dh = q.shape
    C = RET_CHUNK
    N = S // C
    pos = jnp.arange(S)
    q = rotary(q, pos)
    k = rotary(k, pos) * (Dh ** -0.5)
    lg = jnp.log(1.0 - 2.0 ** (-5.0 - jnp.arange(H, dtype=jnp.float32)))
    c = jnp.arange(C, dtype=jnp.float32)
    diff = c[:, None] - c[None, :]
    intra = jnp.where(diff[None] >= 0, jnp.exp(jnp.maximum(diff, 0.0)[None] * lg[:, None, None]), 0.0).astype(q.dtype)
    qc = q.reshape(B, N, C, H, Dh)
    kc = k.reshape(B, N, C, H, Dh)
    vc = v.reshape(B, N, C, H, Dh)
    scores = jnp.einsum('bnihd,bnjhd->bnhij', qc, kc) * intra
    out = jnp.einsum('bnhij,bnjhe->bnihe', scores, vc)
    k_dec = kc * jnp.exp((C - 1 - c)[:, None] * lg[None, :]).astype(q.dtype)[:, :, None]
    kv = jnp.einsum('bnjhd,bnjhe->nbhde', k_dec, vc)
    chunk_decay = jnp.exp(C * lg).astype(kv.dtype)[None, :, None, None]

    def carry_step(state, kv_n):
        return state * chunk_decay + kv_n, state

    _, prev = lax.scan(carry_step, jnp.zeros_like(kv[0]), kv)
    q_dec = qc * jnp.exp((c + 1)[:, None] * lg[None, :]).astype(q.dtype)[:, :, None]
    out = out + jnp.einsum('bnihd,nbhde->bnihe', q_dec, prev)
    out = out.reshape(B, S, H, Dh)
    return head_norm(out, GN_EPS) * gn_gain


def stick_breaking(q, k, v):
    B, S, H, Dh = q.shape
    nb = S // SB_BLOCK
    scale = Dh ** -0.5
    key_pos = jnp.arange(S)
    qb = q.reshape(B, nb, SB_BLOCK, H, Dh).transpose(1, 0, 2, 3, 4)

    def block(args):
        q_blk, b_idx = args
        z = jnp.einsum('bqhd,bkhd->bhqk', q_blk, k).astype(jnp.float32) * scale
        q_pos = b_idx * SB_BLOCK + jnp.arange(SB_BLOCK)
        mask = key_pos[None, :] < q_pos[:, None]
        log_1mb = jnp.where(mask, jax.nn.log_sigmoid(-z), 0.0)
        after = lax.cumsum(log_1mb, axis=3, reverse=True) - log_1mb
        w = jnp.where(mask, jnp.exp(jax.nn.log_sigmoid(z) + after), 0.0)
        return jnp.einsum('bhqk,bkhd->bqhd', w.astype(v.dtype), v)

    out = lax.map(block, (qb, jnp.arange(nb)))
    return out.transpose(1, 0, 2, 3, 4).reshape(B, S, H * Dh)


def even_mixer(hn, w_in, w_out, ret_gn_gain):
    B, S, _ = hn.shape
    z = jnp.einsum('bsd,df->bsf', hn, w_in)
    qa, ka, va, ga, qb, kb, vb, gb = jnp.split(z, 8, axis=-1)
    ra = lambda t: t.reshape(B, S, RET_HEADS, RET_HEAD_DIM)
    rb = lambda t: t.reshape(B, S, SB_HEADS, SB_HEAD_DIM)
    o_a = retention(ra(qa), ra(ka), ra(va), ret_gn_gain).astype(hn.dtype)
    o_b = stick_breaking(rb(qb), rb(kb), rb(vb))
    mixed = jnp.concatenate([o_a * jax.nn.silu(ga), o_b * jax.nn.silu(gb)], axis=-1)
    return jnp.einsum('bsf,fd->bsd', mixed, w_out)


def token_shift(x):
    return jnp.pad(x, ((0, 0), (1, 0), (0, 0)))[:, :-1]


def rwkv7_mixer(hn, mu, w_in, w_out, w0, w1, w2, a0, a1, a2, k_k, k_a, r_k, lnx_g, lnx_b, v_first, vres):
    B, S, D = hn.shape
    H, N = RWKV_HEADS, RWKV_HEAD_DIM
    xx = token_shift(hn) - hn
    xs = hn[None] + xx[None] * mu[:, None, None, :]
    r, k, v, g = jnp.einsum('pbsd,pdf->pbsf', xs[:4], w_in)
    xv, xw, xa = xs[2], xs[4], xs[5]
    w_log = -jax.nn.softplus(-(w0 + jnp.tanh(xw @ w1) @ w2)) - 0.5
    decay = jnp.exp(-jnp.exp(w_log.astype(jnp.float32)))
    a = jax.nn.sigmoid(a0 + (xa @ a1) @ a2)
    if v_first is None:
        v_first = v
    else:
        v0, v1, v2 = vres
        v = v + (v_first - v) * jax.nn.sigmoid(v0 + (xv @ v1) @ v2)
    kk = (k * k_k).reshape(B, S, H, N).astype(jnp.float32)
    kk = kk / jnp.maximum(jnp.linalg.norm(kk, axis=-1, keepdims=True), 1e-12)
    k = k * (1.0 + (a - 1.0) * k_a)
    heads = lambda t: t.reshape(B, S, H, N).astype(jnp.float32)
    rh, kh, vh, ah, wh = heads(r), heads(k), heads(v), heads(a), heads(decay)
    tm = lambda t: t.transpose(1, 0, 2, 3)

    def step(state, inp):
        r_t, w_t, k_t, v_t, kk_t, a_t = inp
        sk = jnp.einsum('bhvk,bhk->bhv', state, kk_t)
        state = state * w_t[:, :, None, :] - sk[..., None] * (kk_t * a_t)[:, :, None, :] + v_t[..., None] * k_t[:, :, None, :]
        return state, jnp.einsum('bhvk,bhk->bhv', state, r_t)

    s0 = jnp.zeros((B, H, N, N), jnp.float32)
    _, ys = lax.scan(step, s0, (tm(rh), tm(wh), tm(kh), tm(vh), tm(kk), tm(ah)))
    y = ys.transpose(1, 0, 2, 3)
    y = head_norm(y, LNX_EPS) * lnx_g + lnx_b
    bonus = jnp.sum(rh * kh * r_k, axis=-1, keepdims=True) * vh
    y = (y + bonus.reshape(B, S, D)).astype(hn.dtype)
    out = jnp.einsum('bsf,fd->bsd', y * jax.nn.silu(g), w_out)
    return out, v_first


def setup_inputs(seed: int = 0) -> dict:
    key = jax.random.key(seed)
    ks = jax.random.split(key, 32)
    nrm = lambda kk, shape, s: jax.random.normal(kk, shape, jnp.float32) * s
    uni = lambda kk, shape: jax.random.uniform(kk, shape, jnp.float32)
    D = D_MODEL
    return {
        "x": nrm(ks[0], (BATCH, SEQ, D), 1.0),
        "p": nrm(ks[1], (DEPTH, BATCH, SEQ, PLE_DIM), 1.0),
        "norm_gain": 1.0 + nrm(ks[2], (DEPTH, D), 0.1),
        "final_gain": 1.0 + nrm(ks[3], (D,), 0.1),
        "ple_proj": nrm(ks[4], (DEPTH, PLE_DIM, D), PLE_DIM ** -0.5),
        "ple_gate": nrm(ks[5], (DEPTH, D, D), D ** -0.5),
        "even_w_in": nrm(ks[6], (N_EVEN, D, EVEN_PROJ), D ** -0.5),
        "even_w_out": nrm(ks[7], (N_EVEN, EVEN_WIDTH, D), EVEN_WIDTH ** -0.5),
        "ret_gn_gain": 1.0 + nrm(ks[8], (N_EVEN, RET_WIDTH), 0.1),
        "odd_mu": uni(ks[9], (N_ODD, 6, D)),
        "odd_w_in": nrm(ks[10], (N_ODD, 4, D, D), D ** -0.5),
        "odd_w_out": nrm(ks[11], (N_ODD, D, D), D ** -0.5),
        "rwkv_w0": -6.0 + 5.0 * uni(ks[12], (N_ODD, D)),
        "rwkv_w1": nrm(ks[13], (N_ODD, D, DECAY_LORA), D ** -0.5),
        "rwkv_w2": nrm(ks[14], (N_ODD, DECAY_LORA, D), 0.1 * DECAY_LORA ** -0.5),
        "rwkv_a0": nrm(ks[15], (N_ODD, D), 0.1),
        "rwkv_a1": nrm(ks[16], (N_ODD, D, ICLR_LORA), D ** -0.5),
        "rwkv_a2": nrm(ks[17], (N_ODD, ICLR_LORA, D), 0.1 * ICLR_LORA ** -0.5),
        "rwkv_v0": 1.0 + nrm(ks[18], (N_VRES, D), 0.1),
        "rwkv_v1": nrm(ks[19], (N_VRES, D, VRES_LORA), D ** -0.5),
        "rwkv_v2": nrm(ks[20], (N_VRES, VRES_LORA, D), 0.1 * VRES_LORA ** -0.5),
        "rwkv_k_k": 0.85 + nrm(ks[21], (N_ODD, D), 0.1),
        "rwkv_k_a": 1.0 + nrm(ks[22], (N_ODD, D), 0.1),
        "rwkv_r_k": nrm(ks[23], (N_ODD, RWKV_HEADS, RWKV_HEAD_DIM), 0.1),
        "rwkv_lnx_gain": 1.0 + nrm(ks[24], (N_ODD, D), 0.1),
        "rwkv_lnx_bias": nrm(ks[25], (N_ODD, D), 0.02),
    }


def reference(x, p, norm_gain, final_gain, ple_proj, ple_gate, even_w_in, even_w_out, ret_gn_gain,
              odd_mu, odd_w_in, odd_w_out, rwkv_w0, rwkv_w1, rwkv_w2, rwkv_a0, rwkv_a1, rwkv_a2,
              rwkv_v0, rwkv_v1, rwkv_v2, rwkv_k_k, rwkv_k_a, rwkv_r_k, rwkv_lnx_gain, rwkv_lnx_bias):
    h = x
    v_first = None
    for i in range(DEPTH):
        hn = rmsnorm(h, norm_gain[i])
        if i % 2 == 0:
            e = i // 2
            h = h + even_mixer(hn, even_w_in[e], even_w_out[e], ret_gn_gain[e])
        else:
            o = i // 2
            vres = None if v_first is None else (rwkv_v0[o - 1], rwkv_v1[o - 1], rwkv_v2[o - 1])
            y, v_first = rwkv7_mixer(hn, odd_mu[o], odd_w_in[o], odd_w_out[o], rwkv_w0[o], rwkv_w1[o], rwkv_w2[o],
                                     rwkv_a0[o], rwkv_a1[o], rwkv_a2[o], rwkv_k_k[o], rwkv_k_a[o], rwkv_r_k[o],
                                     rwkv_lnx_gain[o], rwkv_lnx_bias[o], v_first, vres)
            h = h + y
        gate = jax.nn.sigmoid(jnp.einsum('bsd,de->bse', h, ple_gate[i]))
        h = h + gate * jnp.einsum('bsp,pd->bsd', p[i], ple_proj[i])
    return rmsnorm(h, final_gain)
```

```python
import numpy as np
import concourse.bass as bass
import concourse.mybir as mybir
from concourse.bass_utils import run_bass_kernel_spmd

F32 = mybir.dt.float32
AF = mybir.ActivationFunctionType
ALU = mybir.AluOpType
AX = mybir.AxisListType


class Tl:
    def __init__(self, ap_src, name):
        self.t = ap_src
        self.name = name
        self.st = {}

    def __getitem__(self, k):
        return self.t[k]


class Prog:
    def __init__(self, n_dma_sems=8):
        self.nc = bass.Bass("TRN2", target_bir_lowering=False)
        nc = self.nc
        self.eng = {"pe": nc.tensor, "act": nc.scalar, "dve": nc.vector, "pool": nc.gpsimd, "sp": nc.sync}
        self.sem = {}
        self.cnt = {}
        self._ctx = []
        for e in self.eng:
            s = nc.semaphore("s_" + e)
            self.sem[e] = s.__enter__()
            self._ctx.append(s)
            self.cnt[e] = 0
        self.dma_sems = {}
        for q in ("sp", "pool", "act"):
            lst = []
            for i in range(n_dma_sems):
                s = nc.semaphore(f"d_{q}{i}")
                key = ("dma", q, i)
                self.sem[key] = s.__enter__()
                self._ctx.append(s)
                self.cnt[key] = 0
                lst.append(key)
            self.dma_sems[q] = lst
        self.dma_rr = {"sp": 0, "pool": 0, "act": 0}
        self.seen = {e: {} for e in self.eng}
        self.n_inst = 0
        self._pending = []
        self.attach = True
        self.n_cc = 0
        self.bar_n = 0
        self.bar = {}
        for e in ("pe", "act", "dve", "pool"):
            sg = nc.semaphore("bar_" + e)
            self.bar[e] = sg.__enter__()
        self.n_wait = 0
        self._tid = 0

    def sb(self, shape, dtype=F32, name=None):
        self._tid += 1
        name = name or f"sb{self._tid}"
        shape = list(shape)
        esz = 2 if dtype == mybir.dt.bfloat16 else 4
        nfree = int(np.prod(shape[1:]))
        if (nfree * esz) % 64 != 0:
            assert len(shape) == 2
            pad = ((nfree * esz + 63) // 64) * 64 // esz
            g = self.nc.sbuf_tensor(name, [shape[0], pad], dtype)
            t = g.__enter__()
            self._ctx.append(g)
            return Tl(t[:, 0:shape[1]], name)
        g = self.nc.sbuf_tensor(name, shape, dtype)
        t = g.__enter__()
        self._ctx.append(g)
        return Tl(t, name)

    def ps(self, shape, dtype=F32, name=None):
        self._tid += 1
        name = name or f"ps{self._tid}"
        g = self.nc.psum_tensor(name, list(shape), dtype)
        t = g.__enter__()
        self._ctx.append(g)
        return Tl(t, name)

    def dram(self, name, shape, dtype=F32, kind="ExternalInput"):
        t = self.nc.dram_tensor(name, list(shape), dtype, kind=kind)
        return Tl(t.ap(), name)

    def _need(self, e, ev):
        if ev is None:
            return
        k, v = ev
        if self.seen[e].get(k, 0) >= v:
            return
        self.seen[e][k] = v
        self._pending.append((k, v))
        self.n_wait += 1

    def _emit(self, e, fn):
        pend = self._pending
        self._pending = []
        last = pend.pop() if (pend and self.attach and fn is not None) else None
        for k, v in pend:
            self.eng[e].wait_ge(self.sem[k], v)
        if fn is None:
            return None
        ins = fn(self.eng[e])
        if last is not None:
            ins._wait_ge(self.sem[last[0]], last[1])
        return ins

    def _deps(self, e, reads, writes):
        evs = []
        for (tl, key) in reads:
            for k2, st in tl.st.items():
                if key is None or k2 is None or k2 == key:
                    if st[0] is not None:
                        evs.append(("raw", st[0]))
        for (tl, key) in writes:
            for k2, st in tl.st.items():
                if key is None or k2 is None or k2 == key:
                    if st[0] is not None:
                        evs.append(("waw", st[0]))
                    for r in st[1]:
                        evs.append(("war", r))
        for kind, ev in evs:
            k, v = ev
            if k == e:
                if e == "pe":
                    continue
            self._need(e, ev)

    def _record(self, ev, reads, writes):
        for (tl, key) in reads:
            st = tl.st.setdefault(key, [None, []])
            st[1].append(ev)
            if len(st[1]) > 12:
                best = {}
                for k, v in st[1]:
                    if best.get(k, 0) < v:
                        best[k] = v
                st[1] = list(best.items())
        for (tl, key) in writes:
            if key is None:
                tl.st = {None: [ev, []]}
            else:
                tl.st[key] = [ev, []]

    @staticmethod
    def _norm(lst):
        out = []
        for x in lst:
            if isinstance(x, tuple):
                out.append(x)
            else:
                out.append((x, None))
        return out

    def op(self, e, fn, reads=(), writes=(), inc=True):
        reads = self._norm(reads)
        writes = self._norm(writes)
        self._deps(e, reads, writes)
        ins = self._emit(e, fn)
        if inc:
            self.cnt[e] += 1
            ins.then_inc(self.sem[e], 1)
            ev = (e, self.cnt[e])
        else:
            ev = (e, self.cnt[e] + 1)
        self._record(ev, reads, writes)
        self.n_inst += 1
        return ev

    def dma(self, out_ap, in_ap, reads=(), writes=(), q="sp", **kw):
        reads = self._norm(reads)
        writes = self._norm(writes)
        lst = self.dma_sems[q]
        key = lst[self.dma_rr[q] % len(lst)]
        self.dma_rr[q] += 1
        if self.cnt[key] > 0:
            self._need(q, (key, self.cnt[key]))
        self._deps(q, reads, writes)
        ins = self._emit(q, lambda eng: eng.dma_start(out=out_ap, in_=in_ap, **kw))
        self.cnt[key] += 16
        ins.then_inc(self.sem[key], 16)
        ev = (key, self.cnt[key])
        self._record(ev, reads, writes)
        self.n_inst += 1
        return ev

    def barrier(self):
        self._emit("sp", None)
        for k, v in self.cnt.items():
            if v > 0:
                self._need("sp", (k, v))
        self._emit("sp", None)
        self.bar_n += 1
        for e in ("pe", "act", "dve", "pool"):
            self.eng["sp"].sem_inc(self.bar[e], 1)
            self.eng[e].wait_ge(self.bar[e], self.bar_n)
            for k, v in self.cnt.items():
                self.seen[e][k] = max(self.seen[e].get(k, 0), v)

    def scope_begin(self):
        return len(self._ctx)

    def scope_end(self, mark):
        self.barrier()
        while len(self._ctx) > mark:
            g = self._ctx.pop()
            g.__exit__(None, None, None)

    def cc(self, kind, op, src, dst):
        self._deps("pool", self._norm([src]), self._norm([dst]))
        self._emit("pool", None)
        ins = self.nc.gpsimd.collective_compute(kind, op, replica_groups=[list(range(8))], ins=[src.t.opt()], outs=[dst.t.opt()])
        self.n_cc += 1
        key = ("cc", self.n_cc)
        sg = self.nc.semaphore(f"cc{self.n_cc}")
        self.sem[key] = sg.__enter__()
        self.cnt[key] = 1
        ins.then_inc(self.sem[key])
        self._record((key, 1), self._norm([src]), self._norm([dst]))

    def scratch(self, name, shape, dtype=F32):
        return Tl(self.nc.dram_tensor(name, list(shape), dtype).ap(), name)

    def finish(self, out_tiles):
        for q, lst in self.dma_sems.items():
            for key in lst:
                if self.cnt[key] > 0:
                    self._need("sp", (key, self.cnt[key]))
        for e in ("pe", "act", "dve", "pool"):
            if self.cnt[e] > 0:
                self._need("sp", (e, self.cnt[e]))
        self._emit("sp", None)
        return self.nc


def build_e1(NT=16, stage=2, N=512):
    P = Prog()
    T = NT * 128
    h = P.dram("h", [T, 1024]); gain = P.dram("gain", [128, 8]); w = P.dram("w_in", [1024, 4096])
    ident_d = P.dram("ident", [128, 128])
    z = P.dram("z", [T, 4096], kind="ExternalOutput")
    idt = P.sb([128, 128]); g = P.sb([128, 8])
    hnT = P.sb([128, 8, T])
    P.dma(idt[:], ident_d[:], reads=[ident_d], writes=[idt])
    P.dma(g[:], gain[:], reads=[gain], writes=[g])
    epsc = P.sb([128, 1]); P.op("pool", lambda e: e.memset(epsc[:], 1e-6), writes=[epsc])
    ht = [P.sb([128, 1024]) for _ in range(2)]
    sq = [P.sb([128, 1024]) for _ in range(2)]
    hn = [P.sb([128, 1024]) for _ in range(2)]
    ss = [P.sb([128, 1]) for _ in range(2)]
    rs = [P.sb([128, 1]) for _ in range(2)]
    pt = [P.ps([128, 4, 128]) for _ in range(4)]
    for i in range(NT):
        b = i % 2
        P.dma(ht[b][:], h[i*128:(i+1)*128, :], reads=[h], writes=[ht[b]])
        P.op("act", lambda e: e.activation(out=sq[b][:], in_=ht[b][:], func=AF.Square), reads=[ht[b]], writes=[sq[b]])
        P.op("dve", lambda e: e.reduce_sum(out=ss[b][:], in_=sq[b][:], axis=AX.X), reads=[sq[b]], writes=[ss[b]])
        P.op("act", lambda e: e.activation(out=rs[b][:], in_=ss[b][:], func=AF.Sqrt, scale=1.0/1024, bias=epsc[:]), reads=[ss[b], epsc], writes=[rs[b]])
        P.op("dve", lambda e: e.reciprocal(out=rs[b][:], in_=rs[b][:]), reads=[rs[b]], writes=[rs[b]])
        P.op("dve", lambda e: e.tensor_scalar(out=hn[b][:], in0=ht[b][:], scalar1=rs[b][:, 0:1], scalar2=None, op0=ALU.mult), reads=[ht[b], rs[b]], writes=[hn[b]])
        for half in range(2):
            p_ = pt[(2*i + half) % 4]
            for jj in range(4):
                j = half*4 + jj
                P.op("pe", lambda e: e.transpose(p_[:, jj, :], hn[b][:, j*128:(j+1)*128], idt[:]), reads=[hn[b], idt], writes=[(p_, jj)], inc=(jj == 3))
            for jj in range(4):
                j = half*4 + jj
                eng = "act" if half == 0 else "dve"
                if eng == "act":
                    P.op("act", lambda e: e.activation(out=hnT[:, j, i*128:(i+1)*128], in_=p_[:, jj, :], func=AF.Copy, scale=g[:, j:j+1]), reads=[(p_, jj), g], writes=[(hnT, (j, i))])
                else:
                    P.op("dve", lambda e: e.tensor_scalar(out=hnT[:, j, i*128:(i+1)*128], in0=p_[:, jj, :], scalar1=g[:, j:j+1], scalar2=None, op0=ALU.mult), reads=[(p_, jj), g], writes=[(hnT, (j, i))])
    wv = w.t.rearrange("(j p) c -> p j c", p=128)
    wt = [P.sb([128, 8, 512]) for _ in range(2)]
    pz = [P.ps([128, 512]) for _ in range(4)]
    zo = [P.sb([128, 512]) for _ in range(4)]
    n = 0
    for cg in range(8):
        wb = wt[cg % 2]
        for j in range(8):
            P.dma(wb[:, j, :], w[j*128:(j+1)*128, cg*512:(cg+1)*512], reads=[w], writes=[(wb, j)])
        for i in range(NT):
            pp = pz[n % 4]; oo = zo[n % 4]
            for j in range(8):
                P.op("pe", lambda e: e.matmul(pp[:], lhsT=hnT[:, j, i*128:(i+1)*128], rhs=wb[:, j, :], start=(j == 0), stop=(j == 7)),
                     reads=[(hnT, (j, i)), (wb, j)], writes=[pp], inc=(j == 7))
            if n % 2 == 0:
                P.op("act", lambda e: e.copy(out=oo[:], in_=pp[:]), reads=[pp], writes=[oo])
            else:
                P.op("dve", lambda e: e.tensor_copy(out=oo[:], in_=pp[:]), reads=[pp], writes=[oo])
            P.dma(z[i*128:(i+1)*128, cg*512:(cg+1)*512], oo[:], reads=[oo], writes=[(z, (i, cg))], q="pool")
            n += 1
    P.finish([z])
    return P


def build_post(NT=16, final=False, mode="gated"):
    P = Prog()
    T = NT * 128
    h = P.dram("h", [T, 1024]); m = P.dram("m", [T, 1024]); gin = P.dram("g", [T, 1024]); pin = P.dram("p", [T, 256])
    rw = mode == "rwkv"
    if rw:
        rin = P.dram("r", [T, 1024]); kin = P.dram("k", [T, 1024]); vin = P.dram("v", [T, 1024])
        rowd = {n: P.dram(n, [1, 1024]) for n in ["lnx_g", "lnx_b", "r_k"]}
    w_out = P.dram("w_out", [1024, 1024]); pg = P.dram("ple_gate", [1024, 1024]); pp_w = P.dram("ple_proj", [256, 1024])
    fg = P.dram("fgain", [1, 1024]); ident_d = P.dram("ident", [128, 128])
    out = P.dram("out", [T, 1024], kind="ExternalOutput")
    idt = P.sb([128, 128]); P.dma(idt[:], ident_d[:], reads=[ident_d], writes=[idt])
    wo = P.sb([128, 8, 1024]); wg = P.sb([128, 8, 1024]); wp = P.sb([128, 2, 1024])
    for j in range(8):
        P.dma(wo[:, j, :], w_out[j*128:(j+1)*128, :], reads=[w_out], writes=[(wo, j)])
        P.dma(wg[:, j, :], pg[j*128:(j+1)*128, :], reads=[pg], writes=[(wg, j)])
    for j in range(2):
        P.dma(wp[:, j, :], pp_w[j*128:(j+1)*128, :], reads=[pp_w], writes=[(wp, j)])
    fgb = P.sb([128, 1024])
    epsc = P.sb([128, 1])
    if final:
        P.dma(fgb[:], fg[:].partition_broadcast(128), reads=[fg], writes=[fgb])
        P.op("pool", lambda e: e.memset(epsc[:], 1e-6), writes=[epsc])
    ht = [P.sb([128, 1024]) for _ in range(2)]; mt = [P.sb([128, 1024]) for _ in range(2)]; pt_ = [P.sb([128, 256]) for _ in range(2)]
    gt = [P.sb([128, 1024]) for _ in range(2)]; sg = P.sb([128, 1024])
    if rw:
        rowb = {}
        for n, d in rowd.items():
            rowb[n] = P.sb([128, 1024]); P.dma(rowb[n][:], d[:].partition_broadcast(128), reads=[d], writes=[rowb[n]])
        rt_ = P.sb([128, 1024]); kt_ = P.sb([128, 1024]); vt_ = P.sb([128, 1024]); xc = P.sb([128, 1024]); sq2 = P.sb([128, 1024])
        s16 = P.sb([128, 16]); q16 = P.sb([128, 16]); b16 = P.sb([128, 16]); eps2 = P.sb([128, 1])
        P.op("pool", lambda e: e.memset(eps2[:], 64e-5), writes=[eps2])
    mT = P.sb([128, 8, 128]); h1 = P.sb([128, 1024]); h1T = P.sb([128, 8, 128]); gate = P.sb([128, 1024]); pT = P.sb([128, 2, 128])
    tmp = P.sb([128, 1024]); h2 = [P.sb([128, 1024]) for _ in range(2)]
    sq = P.sb([128, 1024]); ss = P.sb([128, 1]); rs = P.sb([128, 1])
    ptr = [P.ps([128, 4, 128]) for _ in range(2)]
    py = [P.ps([128, 512]) for _ in range(2)]; pgt = [P.ps([128, 512]) for _ in range(2)]; ppp = [P.ps([128, 512]) for _ in range(2)]

    def transposes(src, nch, dstT):
        for g0 in range(0, nch, 4):
            p_ = ptr[(g0 // 4) % 2]
            n = min(4, nch - g0)
            for jj in range(n):
                j = g0 + jj
                P.op("pe", lambda e: e.transpose(p_[:, jj, :], src[:, j*128:(j+1)*128], idt[:]), reads=[src, idt], writes=[p_], inc=(jj == n - 1))
            if (g0 // 4) % 2 == 0:
                P.op("act", lambda e: e.copy(out=dstT[:, g0:g0+n, :], in_=p_[:, 0:n, :]), reads=[p_], writes=[(dstT, g0 // 4)])
            else:
                P.op("dve", lambda e: e.tensor_copy(out=dstT[:, g0:g0+n, :], in_=p_[:, 0:n, :]), reads=[p_], writes=[(dstT, g0 // 4)])

    for i in range(NT):
        b = i % 2
        P.dma(ht[b][:], h[i*128:(i+1)*128, :], reads=[h], writes=[ht[b]])
        P.dma(mt[b][:], m[i*128:(i+1)*128, :], reads=[m], writes=[mt[b]])
        P.dma(pt_[b][:], pin[i*128:(i+1)*128, :], reads=[pin], writes=[pt_[b]])
        P.dma(gt[b][:], gin[i*128:(i+1)*128, :], reads=[gin], writes=[gt[b]])
        P.op("act", lambda e: e.activation(out=sg[:], in_=gt[b][:], func=AF.Silu), reads=[gt[b]], writes=[sg])
        if rw:
            Y = mt[b]; sl_ = slice(i*128, (i+1)*128)
            P.dma(rt_[:], rin[sl_, :], reads=[rin], writes=[rt_]); P.dma(kt_[:], kin[sl_, :], reads=[kin], writes=[kt_]); P.dma(vt_[:], vin[sl_, :], reads=[vin], writes=[vt_])
            v3 = lambda t: t[:].rearrange("p (h f) -> p h f", f=64)
            P.op("dve", lambda e: e.reduce_sum(out=s16[:], in_=v3(Y), axis=AX.X), reads=[Y], writes=[s16])
            P.op("dve", lambda e: e.tensor_scalar(out=s16[:], in0=s16[:], scalar1=-1.0/64, scalar2=None, op0=ALU.mult), reads=[s16], writes=[s16])
            for hh in range(16):
                P.op("dve" if hh % 2 == 0 else "pool", lambda e: e.tensor_scalar(out=xc[:, hh*64:(hh+1)*64], in0=Y[:, hh*64:(hh+1)*64], scalar1=s16[:, hh:hh+1], scalar2=None, op0=ALU.add), reads=[Y, s16], writes=[(xc, hh)])
            P.op("act", lambda e: e.activation(out=sq2[:], in_=xc[:], func=AF.Square), reads=[xc], writes=[sq2])
            P.op("dve", lambda e: e.reduce_sum(out=q16[:], in_=v3(sq2), axis=AX.X), reads=[sq2], writes=[q16])
            P.op("act", lambda e: e.activation(out=q16[:], in_=q16[:], func=AF.Sqrt, scale=1.0/64, bias=eps2[:]), reads=[q16, eps2], writes=[q16])
            P.op("dve", lambda e: e.reciprocal(out=q16[:], in_=q16[:]), reads=[q16], writes=[q16])
            for hh in range(16):
                P.op("dve" if hh % 2 == 0 else "pool", lambda e: e.tensor_scalar(out=xc[:, hh*64:(hh+1)*64], in0=xc[:, hh*64:(hh+1)*64], scalar1=q16[:, hh:hh+1], scalar2=None, op0=ALU.mult), reads=[(xc, hh), q16], writes=[(xc, hh)])
            P.op("dve", lambda e: e.tensor_tensor(out=xc[:], in0=xc[:], in1=rowb["lnx_g"][:], op=ALU.mult), reads=[xc, rowb["lnx_g"]], writes=[xc])
            P.op("pool", lambda e: e.tensor_tensor(out=xc[:], in0=xc[:], in1=rowb["lnx_b"][:], op=ALU.add), reads=[xc, rowb["lnx_b"]], writes=[xc])
            P.op("dve", lambda e: e.tensor_tensor(out=sq2[:], in0=rt_[:], in1=kt_[:], op=ALU.mult), reads=[rt_, kt_], writes=[sq2])
            P.op("pool", lambda e: e.tensor_tensor(out=sq2[:], in0=sq2[:], in1=rowb["r_k"][:], op=ALU.mult), reads=[sq2, rowb["r_k"]], writes=[sq2])
            P.op("dve", lambda e: e.reduce_sum(out=b16[:], in_=v3(sq2), axis=AX.X), reads=[sq2], writes=[b16])
            for hh in range(16):
                P.op("dve", lambda e: e.scalar_tensor_tensor(out=xc[:, hh*64:(hh+1)*64], in0=vt_[:, hh*64:(hh+1)*64], scalar=b16[:, hh:hh+1], in1=xc[:, hh*64:(hh+1)*64], op0=ALU.mult, op1=ALU.add),
                     reads=[vt_, b16, xc], writes=[(xc, hh)])
            P.op("pool", lambda e: e.tensor_tensor(out=mt[b][:], in0=xc[:], in1=sg[:], op=ALU.mult), reads=[xc, sg], writes=[mt[b]])
        else:
            P.op("pool", lambda e: e.tensor_tensor(out=mt[b][:], in0=mt[b][:], in1=sg[:], op=ALU.mult), reads=[mt[b], sg], writes=[mt[b]])
        transposes(mt[b], 8, mT)
        for hf in range(2):
            for j in range(8):
                P.op("pe", lambda e: e.matmul(py[hf][:], lhsT=mT[:, j, :], rhs=wo[:, j, hf*512:(hf+1)*512], start=(j == 0), stop=(j == 7)),
                     reads=[mT, (wo, j)], writes=[py[hf]], inc=(j == 7))
            P.op("dve", lambda e: e.tensor_tensor(out=h1[:, hf*512:(hf+1)*512], in0=py[hf][:], in1=ht[b][:, hf*512:(hf+1)*512], op=ALU.add),
                 reads=[py[hf], ht[b]], writes=[(h1, hf)])
        transposes(h1, 8, h1T)
        for hf in range(2):
            for j in range(8):
                P.op("pe", lambda e: e.matmul(pgt[hf][:], lhsT=h1T[:, j, :], rhs=wg[:, j, hf*512:(hf+1)*512], start=(j == 0), stop=(j == 7)),
                     reads=[h1T, (wg, j)], writes=[pgt[hf]], inc=(j == 7))
            P.op("act", lambda e: e.activation(out=gate[:, hf*512:(hf+1)*512], in_=pgt[hf][:], func=AF.Sigmoid), reads=[pgt[hf]], writes=[(gate, hf)])
        transposes(pt_[b], 2, pT)
        for hf in range(2):
            for j in range(2):
                P.op("pe", lambda e: e.matmul(ppp[hf][:], lhsT=pT[:, j, :], rhs=wp[:, j, hf*512:(hf+1)*512], start=(j == 0), stop=(j == 1)),
                     reads=[pT, (wp, j)], writes=[ppp[hf]], inc=(j == 1))
            P.op("dve", lambda e: e.tensor_tensor(out=tmp[:, hf*512:(hf+1)*512], in0=ppp[hf][:], in1=gate[:, hf*512:(hf+1)*512], op=ALU.mult),
                 reads=[ppp[hf], (gate, hf)], writes=[(tmp, hf)])
        P.op("dve", lambda e: e.tensor_tensor(out=h2[b][:], in0=tmp[:], in1=h1[:], op=ALU.add), reads=[tmp, h1], writes=[h2[b]])
        if final:
            P.op("act", lambda e: e.activation(out=sq[:], in_=h2[b][:], func=AF.Square), reads=[h2[b]], writes=[sq])
            P.op("dve", lambda e: e.reduce_sum(out=ss[:], in_=sq[:], axis=AX.X), reads=[sq], writes=[ss])
            P.op("act", lambda e: e.activation(out=rs[:], in_=ss[:], func=AF.Sqrt, scale=1.0/1024, bias=epsc[:]), reads=[ss, epsc], writes=[rs])
            P.op("dve", lambda e: e.reciprocal(out=rs[:], in_=rs[:]), reads=[rs], writes=[rs])
            P.op("dve", lambda e: e.scalar_tensor_tensor(out=h2[b][:], in0=h2[b][:], scalar=rs[:, 0:1], in1=fgb[:], op0=ALU.mult, op1=ALU.mult),
                 reads=[h2[b], rs, fgb], writes=[h2[b]])
        P.dma(out[i*128:(i+1)*128, :], h2[b][:], reads=[h2[b]], writes=[(out, i)], q="pool")
    P.finish([out])
    return P


def sb_consts():
    j = np.arange(128)
    negtri = np.where(j[:, None] >= j[None, :], -1.0, 0.0).astype(np.float32)
    negones = -np.ones((128, 128), np.float32)
    t = np.arange(512)
    masks = np.stack([((m * 128 + j)[:, None] < t[None, :]).astype(np.float32) for m in range(4)], 0)
    return negtri, negones, masks

def build_sb(S=8192):
    P = Prog()
    NB = S // 128; NG = S // 512
    qd = P.dram("qb", [S, 128]); kd = P.dram("kb", [S, 128]); vd = P.dram("vb", [S, 128])
    ident_d = P.dram("ident", [128, 128]); nt_d = P.dram("negtri", [128, 128]); no_d = P.dram("negones", [128, 128]); mk_d = P.dram("masks", [4, 128, 512])
    outT = P.dram("obT", [128, S], kind="ExternalOutput")
    idt = P.sb([128, 128]); ntr = P.sb([128, 128]); non = P.sb([128, 128]); mk = P.sb([128, 4, 512])
    P.dma(idt[:], ident_d[:], reads=[ident_d], writes=[idt]); P.dma(ntr[:], nt_d[:], reads=[nt_d], writes=[ntr]); P.dma(non[:], no_d[:], reads=[no_d], writes=[non])
    for m in range(4):
        P.dma(mk[:, m, :], mk_d[m], reads=[mk_d], writes=[(mk, m)])
    one = P.sb([128, 1]); P.op("pool", lambda e: e.memset(one[:], 1.0), writes=[one])
    qT = P.sb([128, S]); kT = P.sb([128, S]); v = P.sb([128, NB, 128])
    ptr = [P.ps([128, 4, 128]) for _ in range(2)]
    ld = [P.sb([128, 4, 128]) for _ in range(4)]
    scale = 128 ** -0.5
    n = 0
    for src, dst, sc in ((qd, qT, scale), (kd, kT, 1.0)):
        for g in range(NB // 4):
            lb = ld[n % 4]; p_ = ptr[n % 2]
            P.dma(lb[:], src[g*512:(g+1)*512, :].rearrange("(j p) d -> p j d", p=128), reads=[src], writes=[lb])
            for jj in range(4):
                P.op("pe", lambda e: e.transpose(p_[:, jj, :], lb[:, jj, :], idt[:]), reads=[lb, idt], writes=[p_], inc=(jj == 3))
            if n % 2 == 0:
                P.op("act", lambda e: e.activation(out=dst[:, g*512:(g+1)*512], in_=p_[:].rearrange("p j t -> p (j t)"), func=AF.Copy, scale=sc), reads=[p_], writes=[(dst, g)])
            else:
                P.op("dve", lambda e: e.tensor_scalar(out=dst[:, g*512:(g+1)*512], in0=p_[:].rearrange("p j t -> p (j t)"), scalar1=sc, scalar2=None, op0=ALU.mult), reads=[p_], writes=[(dst, g)])
            n += 1
    for g in range(NB // 4):
        P.dma(v[:, g*4:(g+1)*4, :], vd[g*512:(g+1)*512, :].rearrange("(j p) d -> p j d", p=128), reads=[vd], writes=[(v, g)])
    psA = [P.ps([128, 512]) for _ in range(2)]; psB = [P.ps([128, 512]) for _ in range(2)]; psC = P.ps([128, 512]); psD = P.ps([128, 512])
    Eb = [P.sb([128, 512]) for _ in range(2)]; SPb = [P.sb([128, 512]) for _ in range(2)]; Wb = [P.sb([128, 512]) for _ in range(2)]
    nc_ = [P.sb([128, 512]) for _ in range(2)]; oT = [P.sb([128, 512]) for _ in range(2)]
    Xb = [P.sb([128, 512]) for _ in range(2)]; Tb = [P.sb([128, 512]) for _ in range(2)]
    n = 0
    for G in range(NG):
        qs = qT[:, G*512:(G+1)*512]
        kbs = list(range(4*G + 3, -1, -1))
        ci = 0
        for idx, kb in enumerate(kbs):
            first = (idx == 0); last = (kb == 0); diag = kb >= 4*G; m = kb - 4*G
            A = psA[n % 2]; B = psB[n % 2]; E_ = Eb[n % 2]; SP = SPb[n % 2]; W = Wb[n % 2]
            ks = kT[:, kb*128:(kb+1)*128]
            P.op("pe", lambda e: e.matmul(A[:], lhsT=ks, rhs=qs, start=True, stop=True), reads=[(kT, kb // 4), (qT, G)], writes=[A])
            P.op("act", lambda e: e.activation(out=E_[:], in_=A[:], func=AF.Exp), reads=[A], writes=[E_])
            P.op("act", lambda e: e.activation(out=SP[:], in_=E_[:], func=AF.Ln, bias=one[:]), reads=[E_, one], writes=[SP])
            if diag:
                P.op("dve", lambda e: e.tensor_tensor(out=SP[:], in0=SP[:], in1=mk[:, m, :], op=ALU.mult), reads=[SP, (mk, m)], writes=[SP])
            P.op("pe", lambda e: e.matmul(B[:], lhsT=ntr[:], rhs=SP[:], start=True, stop=True), reads=[ntr, SP], writes=[B])
            X = Xb[n % 2]; T_ = Tb[n % 2]
            if first:
                P.op("act", lambda e: e.activation(out=X[:], in_=B[:], func=AF.Exp), reads=[B], writes=[X])
            else:
                P.op("dve", lambda e: e.tensor_tensor(out=T_[:], in0=B[:], in1=nc_[ci][:], op=ALU.add), reads=[B, nc_[ci]], writes=[T_])
                P.op("act", lambda e: e.activation(out=X[:], in_=T_[:], func=AF.Exp), reads=[T_], writes=[X])
            P.op("pool", lambda e: e.tensor_tensor(out=W[:], in0=E_[:], in1=X[:], op=ALU.mult), reads=[E_, X], writes=[W])
            if diag:
                P.op("pool", lambda e: e.tensor_tensor(out=W[:], in0=W[:], in1=mk[:, m, :], op=ALU.mult), reads=[W, (mk, m)], writes=[W])
            if not last:
                P.op("pe", lambda e: e.matmul(psC[:], lhsT=non[:], rhs=SP[:], start=True, stop=True), reads=[non, SP], writes=[psC])
                if first:
                    P.op("dve", lambda e: e.tensor_copy(out=nc_[0][:], in_=psC[:]), reads=[psC], writes=[nc_[0]]); ci = 0
                else:
                    P.op("dve", lambda e: e.tensor_tensor(out=nc_[1 - ci][:], in0=psC[:], in1=nc_[ci][:], op=ALU.add), reads=[psC, nc_[ci]], writes=[nc_[1 - ci]]); ci = 1 - ci
            P.op("pe", lambda e: e.matmul(psD[:], lhsT=v[:, kb, :], rhs=W[:], start=first, stop=last), reads=[(v, kb // 4), W], writes=[psD], inc=last)
            n += 1
        o_ = oT[G % 2]
        P.op("dve", lambda e: e.tensor_copy(out=o_[:], in_=psD[:]), reads=[psD], writes=[o_])
        P.dma(outT[:, G*512:(G+1)*512], o_[:], reads=[o_], writes=[(outT, G)], q="pool")
    P.finish([outT])
    return P

def sb_ref(q, k, v):
    f8 = np.float64
    q, k, v = q.astype(f8), k.astype(f8), v.astype(f8)
    S = q.shape[0]
    z = (q @ k.T) * 128 ** -0.5
    mask = np.arange(S)[None, :] < np.arange(S)[:, None]
    sp = np.where(mask, np.logaddexp(0, z), 0.0)
    rc = np.cumsum(sp[:, ::-1], axis=1)[:, ::-1]
    w = np.where(mask, np.exp(z - rc), 0.0)
    return w @ v


def ret_consts(head, S):
    f8 = np.float64
    gam = 1.0 - 2.0 ** (-5.0 - head)
    pos = np.arange(S, dtype=f8)
    inv = 10000.0 ** (-np.arange(64, dtype=f8) / 64)
    ang = pos[:, None] * inv[None, :]
    c, s = np.cos(ang), np.sin(ang)
    jj = (np.arange(S) % 128).astype(f8)
    ksc = (128 ** -0.5) * gam ** (-(jj + 1))
    i = np.arange(128)
    mask = (i[:, None] <= i[None, :]).astype(np.float32)
    gq = (gam ** (i + 1.0)).astype(np.float32).reshape(128, 1)
    gC = np.full((128, 1), gam ** 128.0, np.float32)
    return dict(cosq=c.astype(np.float32), sinq=s.astype(np.float32), cosk=(c * ksc[:, None]).astype(np.float32), sink=(s * ksc[:, None]).astype(np.float32),
                rmask=mask, gq=gq, gC=gC)

def build_ret(S=8192):
    P = Prog()
    NC = S // 128
    qd = P.dram("qa", [S, 128]); kd = P.dram("ka", [S, 128]); vd = P.dram("va", [S, 128])
    cq = P.dram("cosq", [S, 64]); sq_ = P.dram("sinq", [S, 64]); ck = P.dram("cosk", [S, 64]); sk = P.dram("sink", [S, 64])
    ident_d = P.dram("ident", [128, 128]); mk_d = P.dram("rmask", [128, 128]); gq_d = P.dram("gq", [128, 1]); gC_d = P.dram("gC", [128, 1]); gn_d = P.dram("gn", [1, 128])
    out = P.dram("oa", [S, 128], kind="ExternalOutput")
    idt = P.sb([128, 128]); mk = P.sb([128, 128]); gq = P.sb([128, 1]); gC = P.sb([128, 1]); gn = P.sb([128, 128])
    P.dma(idt[:], ident_d[:], reads=[ident_d], writes=[idt]); P.dma(mk[:], mk_d[:], reads=[mk_d], writes=[mk])
    P.dma(gq[:], gq_d[:], reads=[gq_d], writes=[gq]); P.dma(gC[:], gC_d[:], reads=[gC_d], writes=[gC])
    P.dma(gn[:], gn_d[:].partition_broadcast(128), reads=[gn_d], writes=[gn])
    eps = P.sb([128, 1]); P.op("pool", lambda e: e.memset(eps[:], 1e-5), writes=[eps])
    St = [P.sb([128, 128]) for _ in range(2)]
    P.op("pool", lambda e: e.memset(St[0][:], 0.0), writes=[St[0]])
    B2 = 2
    qt = [P.sb([128, 128]) for _ in range(B2)]; kt = [P.sb([128, 128]) for _ in range(B2)]; vt = [P.sb([128, 128]) for _ in range(B2)]
    cqt = [P.sb([128, 64]) for _ in range(B2)]; sqt = [P.sb([128, 64]) for _ in range(B2)]; ckt = [P.sb([128, 64]) for _ in range(B2)]; skt = [P.sb([128, 64]) for _ in range(B2)]
    qr = [P.sb([128, 128]) for _ in range(B2)]; kr = [P.sb([128, 128]) for _ in range(B2)]
    ta = P.sb([128, 64]); tb = P.sb([128, 64]); tc = P.sb([128, 64]); td = P.sb([128, 64])
    qkT = P.sb([128, 2, 128]); sTm = P.sb([128, 128]); os_ = P.sb([128, 128]); xc = P.sb([128, 128]); sqq = P.sb([128, 128])
    sm = P.sb([128, 1]); ss = P.sb([128, 1]); rs = P.sb([128, 1]); y = [P.sb([128, 128]) for _ in range(2)]
    ptr = P.ps([128, 2, 128]); ps_s = P.ps([128, 128]); ps_o = P.ps([128, 128]); ps_S = P.ps([128, 128])

    def rotary(eng, x, c, s, o, t1, t2):
        E = lambda f, r, w: P.op(eng, f, reads=r, writes=w)
        E(lambda e: e.tensor_tensor(out=t1[:], in0=x[:, 0:64], in1=c[:], op=ALU.mult), [x, c], [t1])
        E(lambda e: e.tensor_tensor(out=t2[:], in0=x[:, 64:128], in1=s[:], op=ALU.mult), [x, s], [t2])
        E(lambda e: e.tensor_tensor(out=o[:, 0:64], in0=t1[:], in1=t2[:], op=ALU.subtract), [t1, t2], [(o, 0)])
        E(lambda e: e.tensor_tensor(out=t1[:], in0=x[:, 0:64], in1=s[:], op=ALU.mult), [x, s], [t1])
        E(lambda e: e.tensor_tensor(out=t2[:], in0=x[:, 64:128], in1=c[:], op=ALU.mult), [x, c], [t2])
        E(lambda e: e.tensor_tensor(out=o[:, 64:128], in0=t1[:], in1=t2[:], op=ALU.add), [t1, t2], [(o, 1)])

    for c in range(NC):
        b = c % B2; sl = slice(c*128, (c+1)*128)
        for (dst, src) in ((qt[b], qd), (kt[b], kd), (vt[b], vd), (cqt[b], cq), (sqt[b], sq_), (ckt[b], ck), (skt[b], sk)):
            P.dma(dst[:], src[sl, :], reads=[src], writes=[dst])
        rotary("dve", qt[b], cqt[b], sqt[b], qr[b], ta, tb)
        rotary("pool", kt[b], ckt[b], skt[b], kr[b], tc, td)
        P.op("pe", lambda e: e.transpose(ptr[:, 0, :], qr[b][:], idt[:]), reads=[qr[b], idt], writes=[ptr], inc=False)
        P.op("pe", lambda e: e.transpose(ptr[:, 1, :], kr[b][:], idt[:]), reads=[kr[b], idt], writes=[ptr])
        P.op("dve", lambda e: e.tensor_copy(out=qkT[:], in_=ptr[:]), reads=[ptr], writes=[qkT])
        P.op("pe", lambda e: e.matmul(ps_s[:], lhsT=qkT[:, 1, :], rhs=qkT[:, 0, :], start=True, stop=True), reads=[qkT], writes=[ps_s])
        P.op("dve", lambda e: e.tensor_tensor(out=sTm[:], in0=ps_s[:], in1=mk[:], op=ALU.mult), reads=[ps_s, mk], writes=[sTm])
        Sc = St[c % 2]; Sn = St[(c + 1) % 2]
        P.op("pe", lambda e: e.matmul(ps_o[:], lhsT=sTm[:], rhs=vt[b][:], start=True, stop=False), reads=[sTm, vt[b]], writes=[ps_o], inc=False)
        P.op("pe", lambda e: e.matmul(ps_o[:], lhsT=qkT[:, 0, :], rhs=Sc[:], start=False, stop=True), reads=[qkT, Sc], writes=[ps_o])
        P.op("act", lambda e: e.activation(out=os_[:], in_=ps_o[:], func=AF.Copy, scale=gq[:, 0:1]), reads=[ps_o, gq], writes=[os_])
        P.op("pe", lambda e: e.matmul(ps_S[:], lhsT=kr[b][:], rhs=vt[b][:], start=True, stop=False), reads=[kr[b], vt[b]], writes=[ps_S], inc=False)
        P.op("pe", lambda e: e.matmul(ps_S[:], lhsT=idt[:], rhs=Sc[:], start=False, stop=True), reads=[idt, Sc], writes=[ps_S])
        P.op("act", lambda e: e.activation(out=Sn[:], in_=ps_S[:], func=AF.Copy, scale=gC[:, 0:1]), reads=[ps_S, gC], writes=[Sn])
        P.op("dve", lambda e: e.reduce_sum(out=sm[:], in_=os_[:], axis=AX.X), reads=[os_], writes=[sm])
        P.op("dve", lambda e: e.tensor_scalar(out=sm[:], in0=sm[:], scalar1=-1.0/128, scalar2=None, op0=ALU.mult), reads=[sm], writes=[sm])
        P.op("dve", lambda e: e.tensor_scalar(out=xc[:], in0=os_[:], scalar1=sm[:, 0:1], scalar2=None, op0=ALU.add), reads=[os_, sm], writes=[xc])
        P.op("act", lambda e: e.activation(out=sqq[:], in_=xc[:], func=AF.Square), reads=[xc], writes=[sqq])
        P.op("dve", lambda e: e.reduce_sum(out=ss[:], in_=sqq[:], axis=AX.X), reads=[sqq], writes=[ss])
        P.op("act", lambda e: e.activation(out=rs[:], in_=ss[:], func=AF.Sqrt, scale=1.0/128, bias=eps[:]), reads=[ss, eps], writes=[rs])
        P.op("dve", lambda e: e.reciprocal(out=rs[:], in_=rs[:]), reads=[rs], writes=[rs])
        yy = y[c % 2]
        P.op("dve", lambda e: e.scalar_tensor_tensor(out=yy[:], in0=xc[:], scalar=rs[:, 0:1], in1=gn[:], op0=ALU.mult, op1=ALU.mult), reads=[xc, rs, gn], writes=[yy])
        P.dma(out[sl, :], yy[:], reads=[yy], writes=[(out, c)], q="pool")
    P.finish([out])
    return P

def ret_ref(q, k, v, head, gn):
    f8 = np.float64
    q, k, v = q.astype(f8), k.astype(f8), v.astype(f8)
    S = q.shape[0]
    gam = 1.0 - 2.0 ** (-5.0 - head)
    pos = np.arange(S, dtype=f8); inv = 10000.0 ** (-np.arange(64, dtype=f8) / 64); ang = pos[:, None] * inv[None, :]
    c, s = np.cos(ang), np.sin(ang)
    rot = lambda x: np.concatenate([x[:, :64] * c - x[:, 64:] * s, x[:, :64] * s + x[:, 64:] * c], -1)
    q = rot(q); k = rot(k) * 128 ** -0.5
    out = np.zeros((S, 128)); St = np.zeros((128, 128))
    i = np.arange(128)
    D = np.where(i[:, None] >= i[None, :], gam ** np.maximum(i[:, None] - i[None, :], 0).astype(f8), 0.0)
    for n in range(S // 128):
        sl = slice(n*128, (n+1)*128)
        out[sl] = ((q[sl] @ k[sl].T) * D) @ v[sl] + (q[sl] * (gam ** (i + 1.0))[:, None]) @ St
        St = St * gam ** 128 + (k[sl] * (gam ** (127.0 - i))[:, None]).T @ v[sl]
    mu = out.mean(-1, keepdims=True); var = ((out - mu) ** 2).mean(-1, keepdims=True)
    return (out - mu) / np.sqrt(var + 1e-5) * gn


def build_o1(NT=16, vres=False):
    P = Prog()
    T = NT * 128
    h = P.dram("h", [T, 1024]); hprev = P.dram("hprev", [1, 1024]); gain = P.dram("gain", [128, 8]); mu = P.dram("mu", [128, 48])
    w_in = P.dram("w_in", [4, 1024, 1024])
    w1 = P.dram("w1", [1024, 64]); w2 = P.dram("w2", [64, 1024]); a1 = P.dram("a1", [1024, 64]); a2 = P.dram("a2", [64, 1024])
    v1 = P.dram("v1", [1024, 32]); v2 = P.dram("v2", [32, 1024]); vfirst = P.dram("vfirst", [T, 1024])
    rows = {n: P.dram(n, [1, 1024]) for n in ["w0", "a0", "v0", "k_k", "k_a"]}
    ident_d = P.dram("ident", [128, 128])
    outs = {n: P.dram("o_" + n, [T, 1024], kind="ExternalOutput") for n in ["r", "lw", "k", "v", "kk", "b", "g"]}
    idt = P.sb([128, 128]); P.dma(idt[:], ident_d[:], reads=[ident_d], writes=[idt])
    g = P.sb([128, 8]); P.dma(g[:], gain[:], reads=[gain], writes=[g])
    mus = P.sb([128, 48]); P.dma(mus[:], mu[:], reads=[mu], writes=[mus])
    omu = P.sb([128, 48]); P.op("dve", lambda e: e.tensor_scalar(out=omu[:], in0=mus[:], scalar1=-1.0, scalar2=1.0, op0=ALU.mult, op1=ALU.add), reads=[mus], writes=[omu])
    rb = {}
    for n, d in rows.items():
        if n == "v0" and not vres:
            continue
        rb[n] = P.sb([128, 1024]); P.dma(rb[n][:], d[:].partition_broadcast(128), reads=[d], writes=[rb[n]])
    w1s = P.sb([128, 8, 64]); a1s = P.sb([128, 8, 64]); w2s = P.sb([64, 1024]); a2s = P.sb([64, 1024])
    P.dma(w1s[:], w1[:].rearrange("(j p) c -> p j c", p=128), reads=[w1], writes=[w1s]); P.dma(a1s[:], a1[:].rearrange("(j p) c -> p j c", p=128), reads=[a1], writes=[a1s])
    P.dma(w2s[:], w2[:], reads=[w2], writes=[w2s]); P.dma(a2s[:], a2[:], reads=[a2], writes=[a2s])
    if vres:
        v1s = P.sb([128, 8, 32]); v2s = P.sb([32, 1024])
        P.dma(v1s[:], v1[:].rearrange("(j p) c -> p j c", p=128), reads=[v1], writes=[v1s]); P.dma(v2s[:], v2[:], reads=[v2], writes=[v2s])
    epsc = P.sb([128, 1]); P.op("pool", lambda e: e.memset(epsc[:], 1e-6), writes=[epsc])
    one = P.sb([128, 1]); P.op("pool", lambda e: e.memset(one[:], 1.0), writes=[one])
    mhalf = P.sb([128, 1]); P.op("pool", lambda e: e.memset(mhalf[:], -0.5), writes=[mhalf])
    ht = [P.sb([128, 1024]) for _ in range(2)]; sq = P.sb([128, 1024]); hn = P.sb([128, 1024]); ss = P.sb([128, 1]); rs = P.sb([128, 1])
    hnT = [P.sb([128, 8, 132]) for _ in range(2)]
    xs = [P.sb([128, 8, 128]) for _ in range(2)]; tmpx = [P.sb([128, 8, 128]) for _ in range(2)]
    wbuf = [P.sb([128, 8, 512]) for _ in range(2)]
    ptr = [P.ps([128, 4, 128]) for _ in range(2)]; pacc = [P.ps([128, 512]) for _ in range(2)]; pl1 = P.ps([128, 128]); pl2 = [P.ps([128, 512]) for _ in range(2)]
    l1T = P.sb([64, 128])
    A_ = P.sb([128, 1024]); kraw = P.sb([128, 1024]); kkr = P.sb([128, 1024]); t1 = P.sb([128, 1024]); t2 = P.sb([128, 1024])
    ssk = P.sb([128, 16]); rn = P.sb([128, 16])
    ob = {n: [P.sb([128, 1024]) for _ in range(2)] for n in ["r", "v", "g"]}
    for n in ["lw", "k", "kk", "b"]:
        _t = P.sb([128, 1024]); ob[n] = [_t, _t]
    vf = P.sb([128, 1024]); sgv = P.sb([128, 1024])
    wn = [0]

    def norm_T(src, dst, cols):
        n = cols.stop - cols.start
        P.op("act", lambda e: e.activation(out=sq[:], in_=src[:], func=AF.Square), reads=[src], writes=[sq])
        P.op("dve", lambda e: e.reduce_sum(out=ss[:], in_=sq[:], axis=AX.X), reads=[sq], writes=[ss])
        P.op("act", lambda e: e.activation(out=rs[:], in_=ss[:], func=AF.Sqrt, scale=1.0/1024, bias=epsc[:]), reads=[ss, epsc], writes=[rs])
        P.op("dve", lambda e: e.reciprocal(out=rs[:], in_=rs[:]), reads=[rs], writes=[rs])
        P.op("dve", lambda e: e.tensor_scalar(out=hn[:], in0=src[:], scalar1=rs[:, 0:1], scalar2=None, op0=ALU.mult), reads=[src, rs], writes=[hn])
        for half in range(2):
            p_ = ptr[half]
            for jj in range(4):
                j = half*4 + jj
                P.op("pe", lambda e: e.transpose(p_[:, jj, :], hn[:, j*128:(j+1)*128], idt[:]), reads=[hn, idt], writes=[p_], inc=(jj == 3))
            for jj in range(4):
                j = half*4 + jj
                if half == 0:
                    P.op("act", lambda e: e.activation(out=dst[:, j, cols], in_=p_[:, jj, 0:n], func=AF.Copy, scale=g[:, j:j+1]), reads=[p_, g], writes=[(dst, "c")])
                else:
                    P.op("dve", lambda e: e.tensor_scalar(out=dst[:, j, cols], in0=p_[:, jj, 0:n], scalar1=g[:, j:j+1], scalar2=None, op0=ALU.mult), reads=[p_, g], writes=[(dst, "c")])

    P.op("pool", lambda e: e.memset(ht[1][:], 0.0), writes=[ht[1]])
    P.dma(ht[1][0:1, :], hprev[:], reads=[hprev], writes=[ht[1]])
    norm_T(ht[1], hnT[0], slice(0, 1))
    for i in range(NT):
        b = i % 2; HT = hnT[b]; sl = slice(i*128, (i+1)*128)
        P.dma(ht[b][:], h[sl, :], reads=[h], writes=[ht[b]])
        if vres:
            P.dma(vf[:], vfirst[sl, :], reads=[vfirst], writes=[vf])
        norm_T(ht[b], HT, slice(1, 129))
        P.op("pool", lambda e: e.tensor_copy(out=hnT[1 - b][:, :, 0:1], in_=HT[:, :, 128:129]), reads=[HT], writes=[(hnT[1 - b], "p")])
        xi = 0
        for p in (5, 1, 4, 2, 0, 3):
            X = xs[xi % 2]; TX = tmpx[xi % 2]; xi += 1
            for j in range(8):
                P.op("pool", lambda e: e.tensor_scalar(out=TX[:, j, :], in0=HT[:, j, 0:128], scalar1=mus[:, p*8+j:p*8+j+1], scalar2=None, op0=ALU.mult), reads=[HT, mus], writes=[(TX, j)])
                P.op("dve", lambda e: e.scalar_tensor_tensor(out=X[:, j, :], in0=HT[:, j, 1:129], scalar=omu[:, p*8+j:p*8+j+1], in1=TX[:, j, :], op0=ALU.mult, op1=ALU.add),
                     reads=[HT, omu, (TX, j)], writes=[(X, j)])
            lora = None
            if p == 5: lora = (a1s, a2s, 64, "a")
            if p == 4: lora = (w1s, w2s, 64, "w")
            if p == 2 and vres: lora = (v1s, v2s, 32, "v")
            if p < 4:
                for half in range(2):
                    wb = wbuf[wn[0] % 2]; wn[0] += 1
                    for j in range(8):
                        P.dma(wb[:, j, :], w_in[p, j*128:(j+1)*128, half*512:(half+1)*512], reads=[w_in], writes=[(wb, j)])
                    pa = pacc[half]
                    for j in range(8):
                        P.op("pe", lambda e: e.matmul(pa[:], lhsT=X[:, j, :], rhs=wb[:, j, :], start=(j == 0), stop=(j == 7)), reads=[(X, j), (wb, j)], writes=[pa], inc=(j == 7))
                    hsl = slice(half*512, (half+1)*512)
                    if p == 0:
                        P.op("act", lambda e: e.copy(out=ob["r"][b][:, hsl], in_=pa[:]), reads=[pa], writes=[(ob["r"][b], half)])
                    elif p == 3:
                        P.op("act", lambda e: e.copy(out=ob["g"][b][:, hsl], in_=pa[:]), reads=[pa], writes=[(ob["g"][b], half)])
                    elif p == 1:
                        P.op("act", lambda e: e.copy(out=kraw[:, hsl], in_=pa[:]), reads=[pa], writes=[(kraw, half)])
                    elif p == 2:
                        P.op("act", lambda e: e.copy(out=ob["v"][b][:, hsl], in_=pa[:]), reads=[pa], writes=[(ob["v"][b], half)])
            if lora is not None:
                l1w, l2w, R, kind = lora
                for j in range(8):
                    P.op("pe", lambda e: e.matmul(pl1[0:R, :], lhsT=l1w[:, j, :], rhs=X[:, j, :], start=(j == 0), stop=(j == 7)), reads=[l1w, (X, j)], writes=[pl1], inc=(j == 7))
                if kind == "w":
                    P.op("act", lambda e: e.activation(out=l1T[0:R, :], in_=pl1[0:R, :], func=AF.Tanh), reads=[pl1], writes=[l1T])
                else:
                    P.op("act", lambda e: e.copy(out=l1T[0:R, :], in_=pl1[0:R, :]), reads=[pl1], writes=[l1T])
                for half in range(2):
                    hsl = slice(half*512, (half+1)*512)
                    P.op("pe", lambda e: e.matmul(pl2[half][:], lhsT=l1T[0:R, :], rhs=l2w[:, hsl], start=True, stop=True), reads=[l1T, l2w], writes=[pl2[half]])
                    if kind == "a":
                        P.op("dve", lambda e: e.tensor_tensor(out=t1[:, hsl], in0=pl2[half][:], in1=rb["a0"][:, hsl], op=ALU.add), reads=[pl2[half], rb["a0"]], writes=[(t1, half)])
                        P.op("act", lambda e: e.activation(out=A_[:, hsl], in_=t1[:, hsl], func=AF.Sigmoid), reads=[(t1, half)], writes=[(A_, half)])
                    elif kind == "w":
                        P.op("dve", lambda e: e.tensor_tensor(out=t1[:, hsl], in0=pl2[half][:], in1=rb["w0"][:, hsl], op=ALU.add), reads=[pl2[half], rb["w0"]], writes=[(t1, half)])
                        P.op("act", lambda e: e.activation(out=t2[:, hsl], in_=t1[:, hsl], func=AF.Exp, scale=-1.0), reads=[(t1, half)], writes=[(t2, half)])
                        P.op("act", lambda e: e.activation(out=t1[:, hsl], in_=t2[:, hsl], func=AF.Ln, bias=one[:]), reads=[(t2, half), one], writes=[(t1, half)])
                        P.op("act", lambda e: e.activation(out=t2[:, hsl], in_=t1[:, hsl], func=AF.Exp, scale=-1.0, bias=mhalf[:]), reads=[(t1, half), mhalf], writes=[(t2, half)])
                        P.op("dve", lambda e: e.tensor_scalar(out=ob["lw"][b][:, hsl], in0=t2[:, hsl], scalar1=-1.0, scalar2=None, op0=ALU.mult), reads=[(t2, half)], writes=[(ob["lw"][b], half)])
                    else:
                        P.op("dve", lambda e: e.tensor_tensor(out=t1[:, hsl], in0=pl2[half][:], in1=rb["v0"][:, hsl], op=ALU.add), reads=[pl2[half], rb["v0"]], writes=[(t1, half)])
                        P.op("act", lambda e: e.activation(out=sgv[:, hsl], in_=t1[:, hsl], func=AF.Sigmoid), reads=[(t1, half)], writes=[(sgv, half)])
            if p == 1:
                KK = ob["kk"][b]
                P.op("dve", lambda e: e.tensor_tensor(out=kkr[:], in0=kraw[:], in1=rb["k_k"][:], op=ALU.mult), reads=[kraw, rb["k_k"]], writes=[kkr])
                P.op("pool", lambda e: e.tensor_tensor(out=t1[:], in0=kkr[:], in1=kkr[:], op=ALU.mult), reads=[kkr], writes=[t1])
                P.op("dve", lambda e: e.reduce_sum(out=ssk[:], in_=t1[:].rearrange("p (h f) -> p h f", f=64), axis=AX.X), reads=[t1], writes=[ssk])
                P.op("act", lambda e: e.activation(out=rn[:], in_=ssk[:], func=AF.Sqrt), reads=[ssk], writes=[rn])
                P.op("dve", lambda e: e.tensor_scalar(out=rn[:], in0=rn[:], scalar1=1e-12, scalar2=None, op0=ALU.max), reads=[rn], writes=[rn])
                P.op("dve", lambda e: e.reciprocal(out=rn[:], in_=rn[:]), reads=[rn], writes=[rn])
                for hh in range(16):
                    eng = "dve" if hh % 2 == 0 else "pool"
                    P.op(eng, lambda e: e.tensor_scalar(out=KK[:, hh*64:(hh+1)*64], in0=kkr[:, hh*64:(hh+1)*64], scalar1=rn[:, hh:hh+1], scalar2=None, op0=ALU.mult), reads=[kkr, rn], writes=[(KK, hh)])
                P.op("dve", lambda e: e.scalar_tensor_tensor(out=t2[:], in0=A_[:], scalar=-1.0, in1=rb["k_a"][:], op0=ALU.add, op1=ALU.mult), reads=[A_, rb["k_a"]], writes=[t2])
                P.op("dve", lambda e: e.scalar_tensor_tensor(out=ob["k"][b][:], in0=t2[:], scalar=1.0, in1=kraw[:], op0=ALU.add, op1=ALU.mult), reads=[t2, kraw], writes=[ob["k"][b]])
                P.op("pool", lambda e: e.tensor_tensor(out=ob["b"][b][:], in0=KK[:], in1=A_[:], op=ALU.mult), reads=[KK, A_], writes=[ob["b"][b]])
                for n in ("k", "kk", "b"):
                    P.dma(outs[n][sl, :], ob[n][b][:], reads=[ob[n][b]], writes=[(outs[n], i)], q="pool")
            if p == 4:
                P.dma(outs["lw"][sl, :], ob["lw"][b][:], reads=[ob["lw"][b]], writes=[(outs["lw"], i)], q="pool")
            if p == 2:
                if vres:
                    V = ob["v"][b]
                    P.op("pool", lambda e: e.tensor_tensor(out=t1[:], in0=vf[:], in1=V[:], op=ALU.subtract), reads=[vf, V], writes=[t1])
                    P.op("dve", lambda e: e.tensor_tensor(out=t1[:], in0=t1[:], in1=sgv[:], op=ALU.mult), reads=[t1, sgv], writes=[t1])
                    P.op("dve", lambda e: e.tensor_tensor(out=V[:], in0=V[:], in1=t1[:], op=ALU.add), reads=[V, t1], writes=[V])
                P.dma(outs["v"][sl, :], ob["v"][b][:], reads=[ob["v"][b]], writes=[(outs["v"], i)], q="pool")
            if p == 0:
                P.dma(outs["r"][sl, :], ob["r"][b][:], reads=[ob["r"][b]], writes=[(outs["r"], i)], q="pool")
            if p == 3:
                P.dma(outs["g"][sl, :], ob["g"][b][:], reads=[ob["g"][b]], writes=[(outs["g"], i)], q="pool")
    P.finish(list(outs.values()))
    return P

def o1_inputs(inp, o, i_layer, hsh, hprev, vfirst_sh, vres):
    f32 = np.float32; A = lambda a: np.ascontiguousarray(a, dtype=f32)
    d = dict(h=hsh, hprev=hprev, gain=A(inp["norm_gain"][i_layer].reshape(8, 128).T),
             mu=A(inp["odd_mu"][o].reshape(6, 8, 128).transpose(2, 0, 1).reshape(128, 48)),
             w_in=A(inp["odd_w_in"][o]), w1=A(inp["rwkv_w1"][o]), w2=A(inp["rwkv_w2"][o]), a1=A(inp["rwkv_a1"][o]), a2=A(inp["rwkv_a2"][o]),
             w0=A(inp["rwkv_w0"][o]).reshape(1, 1024), a0=A(inp["rwkv_a0"][o]).reshape(1, 1024), k_k=A(inp["rwkv_k_k"][o]).reshape(1, 1024), k_a=A(inp["rwkv_k_a"][o]).reshape(1, 1024),
             ident=np.eye(128, dtype=f32))
    if vres:
        d.update(v1=A(inp["rwkv_v1"][o - 1]), v2=A(inp["rwkv_v2"][o - 1]), v0=A(inp["rwkv_v0"][o - 1]).reshape(1, 1024), vfirst=vfirst_sh)
    else:
        d.update(v1=np.zeros((1024, 32), f32), v2=np.zeros((32, 1024), f32), v0=np.zeros((1, 1024), f32), vfirst=np.zeros_like(hsh))
    return d


def o2_consts():
    i = np.arange(128)
    f = lambda m: np.ascontiguousarray(m.astype(np.float32))
    tri_incl = f(i[:, None] <= i[None, :])
    tri_excl = f(i[:, None] < i[None, :])
    tri_up = f(i[:, None] > i[None, :])
    ones = np.ones((128, 128), np.float32)
    slT = f(i[:, None] < i[None, :])
    ilT = f(i[:, None] <= i[None, :])
    sl = f(i[None, :] < i[:, None])
    m1 = np.concatenate([-slT, ilT], 1); m2 = np.concatenate([slT, ilT], 1)
    return dict(tri_incl=tri_incl, tri_excl=tri_excl, tri_up=tri_up, ones=ones,
                maskM1=np.concatenate([m1, m1], 1), maskM2=np.concatenate([m2, m2], 1), negsl4=np.concatenate([-sl] * 4, 1),
                identrep=np.concatenate([np.eye(64, dtype=np.float32)] * 4, 1), ident4=np.concatenate([np.eye(128, dtype=np.float32)] * 4, 1),
                ident=np.eye(128, dtype=np.float32))

def build_o2_body(P, din, yout, cdram, ident_d, S):
    NCH = S // 128
    names = ["r", "lw", "k", "v", "kk", "b"]
    cn = {}
    cshape = dict(tri_incl=[128, 128], tri_excl=[128, 128], tri_up=[128, 128], ones=[128, 128], maskM1=[128, 512], maskM2=[128, 512], negsl4=[128, 512],
                  identrep=[64, 256], ident4=[128, 512], ident=[128, 128])
    for n_, shp in cshape.items():
        d = ident_d if n_ == "ident" else cdram[n_]
        t = P.sb(shp); P.dma(t[:], d[:], reads=[d], writes=[t]); cn[n_] = t
    idt = cn["ident"]
    bank = [P.ps([128, 512]) for _ in range(8)]
    inb = [{n: P.sb([128, 256]) for n in names} for _ in range(2)]
    ex = {n: P.sb([128, 256]) for n in ["pos", "neg", "prev", "hat"]}
    etot = P.sb([64, 256]); diagG = P.sb([64, 256])
    sc = {n: P.sb([128, 256]) for n in ["rt", "kkt", "bt", "kt", "bh", "kh"]}
    KR = P.sb([64, 4, 2, 128]); BK = P.sb([64, 4, 2, 128])
    M1s = P.sb([128, 4, 256]); M2s = P.sb([128, 4, 256]); Xs = P.sb([128, 4, 128])
    Zp = [P.sb([128, 4, 128]) for _ in range(2)]; Xp = [P.sb([128, 4, 128]) for _ in range(2)]; Tt = [P.sb([128, 4, 128]) for _ in range(2)]
    nr1 = P.sb([128, 256]); U = P.sb([128, 256]); Y = [P.sb([128, 256]) for _ in range(2)]
    ST = [P.sb([64, 256]) for _ in range(2)]
    P.op("pool", lambda e: e.memset(ST[0][:], 0.0), writes=[ST[0]])
    hs = lambda h: slice(h*64, (h+1)*64)
    c4 = lambda h: slice(h*128, (h+1)*128)
    for c in range(NCH):
        ib = inb[c % 2]; sl = slice(c*128, (c+1)*128)
        for n_ in names:
            P.dma(ib[n_][:], din[n_][sl, :], reads=[din[n_]], writes=[ib[n_]])
        P.op("pe", lambda e: e.matmul(bank[0][:, 0:256], lhsT=cn["tri_incl"][:], rhs=ib["lw"][:], start=True, stop=True), reads=[cn["tri_incl"], ib["lw"]], writes=[bank[0]], inc=False)
        P.op("pe", lambda e: e.matmul(bank[0][:, 256:512], lhsT=cn["tri_excl"][:], rhs=ib["lw"][:], start=True, stop=True), reads=[cn["tri_excl"], ib["lw"]], writes=[bank[0]])
        P.op("pe", lambda e: e.matmul(bank[1][:, 0:256], lhsT=cn["tri_up"][:], rhs=ib["lw"][:], start=True, stop=True), reads=[cn["tri_up"], ib["lw"]], writes=[bank[1]], inc=False)
        P.op("pe", lambda e: e.matmul(bank[1][:, 256:512], lhsT=cn["ones"][:], rhs=ib["lw"][:], start=True, stop=True), reads=[cn["ones"], ib["lw"]], writes=[bank[1]])
        P.op("act", lambda e: e.activation(out=ex["pos"][:], in_=bank[0][:, 0:256], func=AF.Exp), reads=[bank[0]], writes=[ex["pos"]])
        P.op("act", lambda e: e.activation(out=ex["neg"][:], in_=bank[0][:, 0:256], func=AF.Exp, scale=-1.0), reads=[bank[0]], writes=[ex["neg"]])
        P.op("act", lambda e: e.activation(out=ex["prev"][:], in_=bank[0][:, 256:512], func=AF.Exp), reads=[bank[0]], writes=[ex["prev"]])
        P.op("act", lambda e: e.activation(out=ex["hat"][:], in_=bank[1][:, 0:256], func=AF.Exp), reads=[bank[1]], writes=[ex["hat"]])
        P.op("act", lambda e: e.activation(out=etot[:], in_=bank[1][0:64, 256:512], func=AF.Exp), reads=[bank[1]], writes=[etot])
        P.op("pool", lambda e: e.tensor_tensor(out=diagG[:], in0=etot[:], in1=cn["identrep"][:], op=ALU.mult), reads=[etot, cn["identrep"]], writes=[diagG])
        for (o_, a_, e_, eng) in (("rt", "r", "pos", "dve"), ("kkt", "kk", "prev", "dve"), ("bt", "b", "neg", "dve"), ("kt", "k", "neg", "dve"), ("bh", "b", "hat", "pool"), ("kh", "k", "hat", "pool")):
            P.op(eng, lambda e: e.tensor_tensor(out=sc[o_][:], in0=ib[a_][:], in1=ex[e_][:], op=ALU.mult), reads=[ib[a_], ex[e_]], writes=[sc[o_]])
        for (dst, pair, b0) in ((KR, ("kkt", "rt"), 2), (BK, ("bt", "kt"), 4)):
            for hp in range(2):
                bk_ = bank[b0 + hp]
                for hh in range(2):
                    h = hp*2 + hh
                    for a in range(2):
                        P.op("pe", lambda e: e.transpose(bk_[0:64, (hh*2+a)*128:(hh*2+a+1)*128], sc[pair[a]][:, hs(h)], idt[:]), reads=[sc[pair[a]], idt], writes=[bk_], inc=(hh == 1 and a == 1))
                eng = "act" if hp == 0 else "dve"
                dv = dst[:, hp*2:(hp+1)*2].rearrange("p h a t -> p (h a t)")
                if eng == "act":
                    P.op("act", lambda e: e.copy(out=dv, in_=bk_[0:64, :]), reads=[bk_], writes=[(dst, hp)])
                else:
                    P.op("dve", lambda e: e.tensor_copy(out=dv, in_=bk_[0:64, :]), reads=[bk_], writes=[(dst, hp)])
        for hp in range(2):
            for hh in range(2):
                h = hp*2 + hh
                P.op("pe", lambda e: e.matmul(bank[hp][:, hh*256:(hh+1)*256], lhsT=BK[:, h, 0, :], rhs=KR[:, h].rearrange("p a t -> p (a t)"), start=True, stop=True), reads=[(BK, hp), (KR, hp)], writes=[bank[hp]], inc=(hh == 1))
            P.op("dve", lambda e: e.tensor_tensor(out=M1s[:, hp*2:(hp+1)*2].rearrange("p h c -> p (h c)"), in0=bank[hp][:], in1=cn["maskM1"][:], op=ALU.mult), reads=[bank[hp], cn["maskM1"]], writes=[(M1s, hp)])
            for hh in range(2):
                h = hp*2 + hh
                P.op("pe", lambda e: e.matmul(bank[6+hp][:, hh*256:(hh+1)*256], lhsT=BK[:, h, 1, :], rhs=KR[:, h].rearrange("p a t -> p (a t)"), start=True, stop=True), reads=[(BK, hp), (KR, hp)], writes=[bank[6+hp]], inc=(hh == 1))
            P.op("dve", lambda e: e.tensor_tensor(out=M2s[:, hp*2:(hp+1)*2].rearrange("p h c -> p (h c)"), in0=bank[6+hp][:], in1=cn["maskM2"][:], op=ALU.mult), reads=[bank[6+hp], cn["maskM2"]], writes=[(M2s, hp)])
        for h in range(4):
            P.op("pe", lambda e: e.matmul(bank[2][:, c4(h)], lhsT=KR[:, h, 0, :], rhs=BK[:, h, 0, :], start=True, stop=True), reads=[KR, BK], writes=[bank[2]], inc=(h == 3))
        P.op("dve", lambda e: e.tensor_tensor(out=Xp[0][:].rearrange("p h t -> p (h t)"), in0=bank[2][:], in1=cn["negsl4"][:], op=ALU.mult), reads=[bank[2], cn["negsl4"]], writes=[Xp[0]])
        P.op("pool", lambda e: e.tensor_copy(out=Zp[0][:], in_=M1s[:, :, 0:128]), reads=[M1s], writes=[Zp[0]])
        P.op("pool", lambda e: e.tensor_tensor(out=Tt[0][:].rearrange("p h t -> p (h t)"), in0=Zp[0][:].rearrange("p h t -> p (h t)"), in1=cn["ident4"][:], op=ALU.add), reads=[Zp[0], cn["ident4"]], writes=[Tt[0]])
        cur = 0
        for it in range(6):
            nx = 1 - cur
            for h in range(4):
                P.op("pe", lambda e: e.matmul(bank[3][:, c4(h)], lhsT=Zp[cur][:, h, :], rhs=Xp[cur][:, h, :], start=True, stop=True), reads=[Zp[cur], Xp[cur]], writes=[bank[3]], inc=(h == 3))
            P.op("act", lambda e: e.copy(out=Xp[nx][:].rearrange("p h t -> p (h t)"), in_=bank[3][:]), reads=[bank[3]], writes=[Xp[nx]])
            if it < 5:
                for h in range(4):
                    P.op("pe", lambda e: e.matmul(bank[4][:, c4(h)], lhsT=Xp[cur][:, h, :], rhs=Zp[cur][:, h, :], start=True, stop=True), reads=[Zp[cur], Xp[cur]], writes=[bank[4]], inc=(h == 3))
                P.op("dve", lambda e: e.tensor_copy(out=Zp[nx][:].rearrange("p h t -> p (h t)"), in_=bank[4][:]), reads=[bank[4]], writes=[Zp[nx]])
            for h in range(4):
                P.op("pe", lambda e: e.matmul(bank[5][:, c4(h)], lhsT=idt[:], rhs=Tt[cur][:, h, :], start=True, stop=False), reads=[idt, Tt[cur]], writes=[bank[5]], inc=False)
                P.op("pe", lambda e: e.matmul(bank[5][:, c4(h)], lhsT=Xp[nx][:, h, :], rhs=Tt[cur][:, h, :], start=False, stop=True), reads=[Xp[nx], Tt[cur]], writes=[bank[5]], inc=(h == 3))
            P.op("dve", lambda e: e.tensor_copy(out=Tt[nx][:].rearrange("p h t -> p (h t)"), in_=bank[5][:]), reads=[bank[5]], writes=[Tt[nx]])
            cur = nx
        TT = Tt[cur]
        Sc = ST[c % 2]; Sn = ST[(c + 1) % 2]; V = ib["v"]
        for h in range(4):
            P.op("pe", lambda e: e.matmul(bank[6][:, hs(h)], lhsT=KR[:, h, 0, :], rhs=Sc[:, hs(h)], start=True, stop=False), reads=[KR, Sc], writes=[bank[6]], inc=False)
            P.op("pe", lambda e: e.matmul(bank[6][:, hs(h)], lhsT=M2s[:, h, 0:128], rhs=V[:, hs(h)], start=False, stop=True), reads=[M2s, V], writes=[bank[6]], inc=(h == 3))
        P.op("act", lambda e: e.activation(out=nr1[:], in_=bank[6][:, 0:256], func=AF.Copy, scale=-1.0), reads=[bank[6]], writes=[nr1])
        for h in range(4):
            P.op("pe", lambda e: e.matmul(bank[7][:, hs(h)], lhsT=TT[:, h, :], rhs=nr1[:, hs(h)], start=True, stop=True), reads=[TT, nr1], writes=[bank[7]], inc=(h == 3))
        P.op("dve", lambda e: e.tensor_copy(out=U[:], in_=bank[7][:, 0:256]), reads=[bank[7]], writes=[U])
        for h in range(4):
            P.op("pe", lambda e: e.matmul(bank[6][:, 256 + h*64:256 + (h+1)*64], lhsT=KR[:, h, 1, :], rhs=Sc[:, hs(h)], start=True, stop=False), reads=[KR, Sc], writes=[bank[6]], inc=False)
            P.op("pe", lambda e: e.matmul(bank[6][:, 256 + h*64:256 + (h+1)*64], lhsT=M1s[:, h, 128:256], rhs=U[:, hs(h)], start=False, stop=False), reads=[M1s, U], writes=[bank[6]], inc=False)
            P.op("pe", lambda e: e.matmul(bank[6][:, 256 + h*64:256 + (h+1)*64], lhsT=M2s[:, h, 128:256], rhs=V[:, hs(h)], start=False, stop=True), reads=[M2s, V], writes=[bank[6]], inc=(h == 3))
        yb = Y[c % 2]
        P.op("act", lambda e: e.copy(out=yb[:], in_=bank[6][:, 256:512]), reads=[bank[6]], writes=[yb])
        P.dma(yout[sl, :], yb[:], reads=[yb], writes=[(yout, c)], q="pool")
        for h in range(4):
            P.op("pe", lambda e: e.matmul(bank[7][0:64, 256 + h*64:256 + (h+1)*64], lhsT=diagG[:, hs(h)], rhs=Sc[:, hs(h)], start=True, stop=False), reads=[diagG, Sc], writes=[bank[7]], inc=False)
            P.op("pe", lambda e: e.matmul(bank[7][0:64, 256 + h*64:256 + (h+1)*64], lhsT=sc["bh"][:, hs(h)], rhs=U[:, hs(h)], start=False, stop=False), reads=[sc["bh"], U], writes=[bank[7]], inc=False)
            P.op("pe", lambda e: e.matmul(bank[7][0:64, 256 + h*64:256 + (h+1)*64], lhsT=sc["kh"][:, hs(h)], rhs=V[:, hs(h)], start=False, stop=True), reads=[sc["kh"], V], writes=[bank[7]], inc=(h == 3))
        P.op("dve", lambda e: e.tensor_copy(out=Sn[:], in_=bank[7][0:64, 256:512]), reads=[bank[7]], writes=[Sn])


def build_o2(S=8192):
    P = Prog()
    names = ["r", "lw", "k", "v", "kk", "b"]
    din = {n: P.dram(n, [S, 256]) for n in names}
    cshape = dict(tri_incl=[128, 128], tri_excl=[128, 128], tri_up=[128, 128], ones=[128, 128], maskM1=[128, 512], maskM2=[128, 512], negsl4=[128, 512],
                  identrep=[64, 256], ident4=[128, 512])
    cdram = {n_: P.dram(n_, shp) for n_, shp in cshape.items()}
    ident_d = P.dram("ident", [128, 128])
    yout = P.dram("y", [S, 256], kind="ExternalOutput")
    build_o2_body(P, din, yout, cdram, ident_d, S)
    P.finish([yout])
    return P


def _run(P, in_maps):
    res = run_bass_kernel_spmd(P.nc, in_maps, core_ids=list(range(8)))
    return res.results


def kernel(**inp):
    f32 = np.float32
    A = lambda a: np.ascontiguousarray(a, dtype=f32)
    x = A(inp["x"]).reshape(16384, 1024)
    p = A(inp["p"]).reshape(4, 16384, 256)
    ident = np.eye(128, dtype=f32)
    negtri, negones, masks = sb_consts()
    o2c = o2_consts()
    S = 8192
    h = x
    v_first = None
    sh = lambda a, c: A(a[c*2048:(c+1)*2048])
    cat = lambda r, n: np.concatenate([r[c][n] for c in range(8)], 0)
    for i in range(4):
        last = (i == 3)
        post_in = [{"h": sh(h, c), "p": sh(p[i], c), "ple_gate": A(inp["ple_gate"][i]), "ple_proj": A(inp["ple_proj"][i]),
                    "fgain": A(inp["final_gain"]).reshape(1, 1024), "ident": ident} for c in range(8)]
        if i % 2 == 0:
            e = i // 2
            g2 = A(inp["norm_gain"][i].reshape(8, 128).T)
            w_in = A(inp["even_w_in"][e])
            r = _run(build_e1(16), [{"h": sh(h, c), "gain": g2, "w_in": w_in, "ident": ident} for c in range(8)])
            z = cat(r, "z").reshape(2, S, 8, 4, 128)
            gnv = A(inp["ret_gn_gain"][e]).reshape(4, 1, 128)
            ims = []
            for c in range(8):
                b, hd = c // 4, c % 4
                d = dict(qa=A(z[b, :, 0, hd]), ka=A(z[b, :, 1, hd]), va=A(z[b, :, 2, hd]), ident=ident, gn=gnv[hd])
                d.update(ret_consts(hd, S))
                ims.append(d)
            r = _run(build_ret(S), ims)
            o_a = np.stack([np.stack([r[b*4+hd]["oa"] for hd in range(4)], 1) for b in range(2)], 0)
            ims = [dict(qb=A(z[c // 4, :, 4, c % 4]), kb=A(z[c // 4, :, 5, c % 4]), vb=A(z[c // 4, :, 6, c % 4]), ident=ident,
                        negtri=negtri, negones=negones, masks=masks) for c in range(8)]
            r = _run(build_sb(S), ims)
            o_b = np.stack([np.stack([r[b*4+hd]["obT"].T for hd in range(4)], 1) for b in range(2)], 0)
            o = A(np.concatenate([o_a.reshape(2, S, 512), o_b.reshape(2, S, 512)], -1).reshape(16384, 1024))
            g = A(np.concatenate([z[:, :, 3].reshape(2, S, 512), z[:, :, 7].reshape(2, S, 512)], -1).reshape(16384, 1024))
            for c in range(8):
                post_in[c].update(m=sh(o, c), g=sh(g, c), w_out=A(inp["even_w_out"][e]))
            mode = "gated"
        else:
            o = i // 2
            vres = v_first is not None
            ims = []
            for c in range(8):
                hprev = np.zeros((1, 1024), f32) if c % 4 == 0 else A(h[c*2048-1:c*2048])
                ims.append(o1_inputs(inp, o, i, sh(h, c), hprev, sh(v_first, c) if vres else None, vres))
            r = _run(build_o1(16, vres), ims)
            q = {n: cat(r, "o_" + n) for n in ["r", "lw", "k", "v", "kk", "b", "g"]}
            if v_first is None:
                v_first = q["v"]
            ims = []
            for c in range(8):
                b, hg = c // 4, c % 4
                d = {n: A(q[n].reshape(2, S, 1024)[b, :, hg*256:(hg+1)*256]) for n in ["r", "lw", "k", "v", "kk", "b"]}
                d.update(o2c)
                ims.append(d)
            r = _run(build_o2(S), ims)
            y = A(np.stack([np.concatenate([r[b*4+hg]["y"] for hg in range(4)], 1) for b in range(2)], 0).reshape(16384, 1024))
            for c in range(8):
                post_in[c].update(m=sh(y, c), g=sh(q["g"], c), r=sh(q["r"], c), k=sh(q["k"], c), v=sh(q["v"], c), w_out=A(inp["odd_w_out"][o]),
                                  lnx_g=A(inp["rwkv_lnx_gain"][o]).reshape(1, 1024), lnx_b=A(inp["rwkv_lnx_bias"][o]).reshape(1, 1024),
                                  r_k=A(inp["rwkv_r_k"][o]).reshape(1, 1024))
            mode = "rwkv"
        r = _run(build_post(16, final=last, mode=mode), post_in)
        h = cat(r, "out")
    return h.reshape(2, S, 1024).astype(f32)
```

```python
import numpy as np
import concourse.bass as bass
import concourse.mybir as mybir
from concourse.bass_utils import run_bass_kernel_spmd

F32 = mybir.dt.float32
AF = mybir.ActivationFunctionType
ALU = mybir.AluOpType
AX = mybir.AxisListType
SAME_ENGINE_ALL = True


class Tl:
    def __init__(self, ap_src, name):
        self.t = ap_src
        self.name = name
        self.st = {}

    def __getitem__(self, k):
        return self.t[k]


class Prog:
    def __init__(self, n_dma_sems=8):
        self.nc = bass.Bass("TRN2", target_bir_lowering=False)
        nc = self.nc
        self.eng = {"pe": nc.tensor, "act": nc.scalar, "dve": nc.vector, "pool": nc.gpsimd, "sp": nc.sync}
        self.sem = {}
        self.cnt = {}
        self._ctx = []
        for e in self.eng:
            s = nc.semaphore("s_" + e)
            self.sem[e] = s.__enter__()
            self._ctx.append(s)
            self.cnt[e] = 0
        self.dma_sems = {}
        for q in ("sp", "pool", "act"):
            lst = []
            for i in range(n_dma_sems):
                s = nc.semaphore(f"d_{q}{i}")
                key = ("dma", q, i)
                self.sem[key] = s.__enter__()
                self._ctx.append(s)
                self.cnt[key] = 0
                lst.append(key)
            self.dma_sems[q] = lst
        self.dma_rr = {"sp": 0, "pool": 0, "act": 0}
        self.seen = {e: {} for e in self.eng}
        self.n_inst = 0
        self._pending = []
        self.attach = True
        self.n_cc = 0
        self.bar_n = 0
        self.bar = {}
        for e in ("pe", "act", "dve", "pool"):
            sg = nc.semaphore("bar_" + e)
            self.bar[e] = sg.__enter__()
        self.n_wait = 0
        self._tid = 0

    def sb(self, shape, dtype=F32, name=None):
        self._tid += 1
        name = name or f"sb{self._tid}"
        shape = list(shape)
        esz = 2 if dtype == mybir.dt.bfloat16 else 4
        nfree = int(np.prod(shape[1:]))
        if (nfree * esz) % 64 != 0:
            assert len(shape) == 2
            pad = ((nfree * esz + 63) // 64) * 64 // esz
            g = self.nc.sbuf_tensor(name, [shape[0], pad], dtype)
            t = g.__enter__()
            self._ctx.append(g)
            return Tl(t[:, 0:shape[1]], name)
        g = self.nc.sbuf_tensor(name, shape, dtype)
        t = g.__enter__()
        self._ctx.append(g)
        return Tl(t, name)

    def ps(self, shape, dtype=F32, name=None):
        self._tid += 1
        name = name or f"ps{self._tid}"
        g = self.nc.psum_tensor(name, list(shape), dtype)
        t = g.__enter__()
        self._ctx.append(g)
        return Tl(t, name)

    def dram(self, name, shape, dtype=F32, kind="ExternalInput"):
        t = self.nc.dram_tensor(name, list(shape), dtype, kind=kind)
        return Tl(t.ap(), name)

    def _need(self, e, ev):
        if ev is None:
            return
        k, v = ev
        if self.seen[e].get(k, 0) >= v:
            return
        self.seen[e][k] = v
        self._pending.append((k, v))
        self.n_wait += 1

    def _emit(self, e, fn):
        pend = self._pending
        self._pending = []
        last = pend.pop() if (pend and self.attach and fn is not None) else None
        for k, v in pend:
            self.eng[e].wait_ge(self.sem[k], v)
        if fn is None:
            return None
        ins = fn(self.eng[e])
        if last is not None:
            ins._wait_ge(self.sem[last[0]], last[1])
        return ins

    def _deps(self, e, reads, writes):
        evs = []
        for (tl, key) in reads:
            for k2, st in tl.st.items():
                if key is None or k2 is None or k2 == key:
                    if st[0] is not None:
                        evs.append(("raw", st[0]))
        for (tl, key) in writes:
            for k2, st in tl.st.items():
                if key is None or k2 is None or k2 == key:
                    if st[0] is not None:
                        evs.append(("waw", st[0]))
                    for r in st[1]:
                        evs.append(("war", r))
        for kind, ev in evs:
            k, v = ev
            if k == e:
                if e == "pe" or (kind != "raw" and not SAME_ENGINE_ALL):
                    continue
            self._need(e, ev)

    def _record(self, ev, reads, writes):
        for (tl, key) in reads:
            st = tl.st.setdefault(key, [None, []])
            st[1].append(ev)
            if len(st[1]) > 12:
                best = {}
                for k, v in st[1]:
                    if best.get(k, 0) < v:
                        best[k] = v
                st[1] = list(best.items())
        for (tl, key) in writes:
            if key is None:
                tl.st = {None: [ev, []]}
            else:
                tl.st[key] = [ev, []]

    @staticmethod
    def _norm(lst):
        out = []
        for x in lst:
            if isinstance(x, tuple):
                out.append(x)
            else:
                out.append((x, None))
        return out

    def op(self, e, fn, reads=(), writes=(), inc=True):
        reads = self._norm(reads)
        writes = self._norm(writes)
        self._deps(e, reads, writes)
        ins = self._emit(e, fn)
        if inc:
            self.cnt[e] += 1
            ins.then_inc(self.sem[e], 1)
            ev = (e, self.cnt[e])
        else:
            ev = (e, self.cnt[e] + 1)
        self._record(ev, reads, writes)
        self.n_inst += 1
        return ev

    def dma(self, out_ap, in_ap, reads=(), writes=(), q="sp", **kw):
        reads = self._norm(reads)
        writes = self._norm(writes)
        lst = self.dma_sems[q]
        key = lst[self.dma_rr[q] % len(lst)]
        self.dma_rr[q] += 1
        if self.cnt[key] > 0:
            self._need(q, (key, self.cnt[key]))
        self._deps(q, reads, writes)
        ins = self._emit(q, lambda eng: eng.dma_start(out=out_ap, in_=in_ap, **kw))
        self.cnt[key] += 16
        ins.then_inc(self.sem[key], 16)
        ev = (key, self.cnt[key])
        self._record(ev, reads, writes)
        self.n_inst += 1
        return ev

    def barrier(self):
        self._emit("sp", None)
        for k, v in self.cnt.items():
            if v > 0:
                self._need("sp", (k, v))
        self._emit("sp", None)
        self.bar_n += 1
        for e in ("pe", "act", "dve", "pool"):
            self.eng["sp"].sem_inc(self.bar[e], 1)
            self.eng[e].wait_ge(self.bar[e], self.bar_n)
            for k, v in self.cnt.items():
                self.seen[e][k] = max(self.seen[e].get(k, 0), v)

    def scope_begin(self):
        return len(self._ctx)

    def scope_end(self, mark):
        self.barrier()
        while len(self._ctx) > mark:
            g = self._ctx.pop()
            g.__exit__(None, None, None)

    def cc(self, kind, op, src, dst):
        self._deps("pool", self._norm([src]), self._norm([dst]))
        self._emit("pool", None)
        ins = self.nc.gpsimd.collective_compute(kind, op, replica_groups=[list(range(8))], ins=[src.t.opt()], outs=[dst.t.opt()])
        self.n_cc += 1
        key = ("cc", self.n_cc)
        sg = self.nc.semaphore(f"cc{self.n_cc}")
        self.sem[key] = sg.__enter__()
        self.cnt[key] = 1
        ins.then_inc(self.sem[key])
        self._record((key, 1), self._norm([src]), self._norm([dst]))

    def scratch(self, name, shape, dtype=F32):
        return Tl(self.nc.dram_tensor(name, list(shape), dtype).ap(), name)

    def finish(self, out_tiles):
        for q, lst in self.dma_sems.items():
            for key in lst:
                if self.cnt[key] > 0:
                    self._need("sp", (key, self.cnt[key]))
        for e in ("pe", "act", "dve", "pool"):
            if self.cnt[e] > 0:
                self._need("sp", (e, self.cnt[e]))
        self._emit("sp", None)
        return self.nc


def build_e1(NT=16, stage=2, N=512):
    P = Prog()
    T = NT * 128
    h = P.dram("h", [T, 1024]); gain = P.dram("gain", [128, 8]); w = P.dram("w_in", [1024, 4096])
    ident_d = P.dram("ident", [128, 128])
    z = P.dram("z", [T, 4096], kind="ExternalOutput")
    idt = P.sb([128, 128]); g = P.sb([128, 8])
    hnT = P.sb([128, 8, T])
    P.dma(idt[:], ident_d[:], reads=[ident_d], writes=[idt])
    P.dma(g[:], gain[:], reads=[gain], writes=[g])
    epsc = P.sb([128, 1]); P.op("pool", lambda e: e.memset(epsc[:], 1e-6), writes=[epsc])
    ht = [P.sb([128, 1024]) for _ in range(2)]
    sq = [P.sb([128, 1024]) for _ in range(2)]
    hn = [P.sb([128, 1024]) for _ in range(2)]
    ss = [P.sb([128, 1]) for _ in range(2)]
    rs = [P.sb([128, 1]) for _ in range(2)]
    pt = [P.ps([128, 4, 128]) for _ in range(4)]
    for i in range(NT):
        b = i % 2
        P.dma(ht[b][:], h[i*128:(i+1)*128, :], reads=[h], writes=[ht[b]])
        P.op("act", lambda e: e.activation(out=sq[b][:], in_=ht[b][:], func=AF.Square), reads=[ht[b]], writes=[sq[b]])
        P.op("dve", lambda e: e.reduce_sum(out=ss[b][:], in_=sq[b][:], axis=AX.X), reads=[sq[b]], writes=[ss[b]])
        P.op("act", lambda e: e.activation(out=rs[b][:], in_=ss[b][:], func=AF.Sqrt, scale=1.0/1024, bias=epsc[:]), reads=[ss[b], epsc], writes=[rs[b]])
        P.op("dve", lambda e: e.reciprocal(out=rs[b][:], in_=rs[b][:]), reads=[rs[b]], writes=[rs[b]])
        P.op("dve", lambda e: e.tensor_scalar(out=hn[b][:], in0=ht[b][:], scalar1=rs[b][:, 0:1], scalar2=None, op0=ALU.mult), reads=[ht[b], rs[b]], writes=[hn[b]])
        for half in range(2):
            p_ = pt[(2*i + half) % 4]
            for jj in range(4):
                j = half*4 + jj
                P.op("pe", lambda e: e.transpose(p_[:, jj, :], hn[b][:, j*128:(j+1)*128], idt[:]), reads=[hn[b], idt], writes=[(p_, jj)], inc=(jj == 3))
            for jj in range(4):
                j = half*4 + jj
                eng = "act" if half == 0 else "dve"
                if eng == "act":
                    P.op("act", lambda e: e.activation(out=hnT[:, j, i*128:(i+1)*128], in_=p_[:, jj, :], func=AF.Copy, scale=g[:, j:j+1]), reads=[(p_, jj), g], writes=[(hnT, (j, i))])
                else:
                    P.op("dve", lambda e: e.tensor_scalar(out=hnT[:, j, i*128:(i+1)*128], in0=p_[:, jj, :], scalar1=g[:, j:j+1], scalar2=None, op0=ALU.mult), reads=[(p_, jj), g], writes=[(hnT, (j, i))])
    wv = w.t.rearrange("(j p) c -> p j c", p=128)
    wt = [P.sb([128, 8, 512]) for _ in range(2)]
    pz = [P.ps([128, 512]) for _ in range(4)]
    zo = [P.sb([128, 512]) for _ in range(4)]
    n = 0
    for cg in range(8):
        wb = wt[cg % 2]
        for j in range(8):
            P.dma(wb[:, j, :], w[j*128:(j+1)*128, cg*512:(cg+1)*512], reads=[w], writes=[(wb, j)])
        for i in range(NT):
            pp = pz[n % 4]; oo = zo[n % 4]
            for j in range(8):
                P.op("pe", lambda e: e.matmul(pp[:], lhsT=hnT[:, j, i*128:(i+1)*128], rhs=wb[:, j, :], start=(j == 0), stop=(j == 7)),
                     reads=[(hnT, (j, i)), (wb, j)], writes=[pp], inc=(j == 7))
            if n % 2 == 0:
                P.op("act", lambda e: e.copy(out=oo[:], in_=pp[:]), reads=[pp], writes=[oo])
            else:
                P.op("dve", lambda e: e.tensor_copy(out=oo[:], in_=pp[:]), reads=[pp], writes=[oo])
            P.dma(z[i*128:(i+1)*128, cg*512:(cg+1)*512], oo[:], reads=[oo], writes=[(z, (i, cg))], q="pool")
            n += 1
    P.finish([z])
    return P


def build_post(NT=16, final=False, mode="gated"):
    P = Prog()
    T = NT * 128
    h = P.dram("h", [T, 1024]); m = P.dram("m", [T, 1024]); gin = P.dram("g", [T, 1024]); pin = P.dram("p", [T, 256])
    rw = mode == "rwkv"
    if rw:
        rin = P.dram("r", [T, 1024]); kin = P.dram("k", [T, 1024]); vin = P.dram("v", [T, 1024])
        rowd = {n: P.dram(n, [1, 1024]) for n in ["lnx_g", "lnx_b", "r_k"]}
    w_out = P.dram("w_out", [1024, 1024]); pg = P.dram("ple_gate", [1024, 1024]); pp_w = P.dram("ple_proj", [256, 1024])
    fg = P.dram("fgain", [1, 1024]); ident_d = P.dram("ident", [128, 128])
    out = P.dram("out", [T, 1024], kind="ExternalOutput")
    idt = P.sb([128, 128]); P.dma(idt[:], ident_d[:], reads=[ident_d], writes=[idt])
    wo = P.sb([128, 8, 1024]); wg = P.sb([128, 8, 1024]); wp = P.sb([128, 2, 1024])
    for j in range(8):
        P.dma(wo[:, j, :], w_out[j*128:(j+1)*128, :], reads=[w_out], writes=[(wo, j)])
        P.dma(wg[:, j, :], pg[j*128:(j+1)*128, :], reads=[pg], writes=[(wg, j)])
    for j in range(2):
        P.dma(wp[:, j, :], pp_w[j*128:(j+1)*128, :], reads=[pp_w], writes=[(wp, j)])
    fgb = P.sb([128, 1024])
    epsc = P.sb([128, 1])
    if final:
        P.dma(fgb[:], fg[:].partition_broadcast(128), reads=[fg], writes=[fgb])
        P.op("pool", lambda e: e.memset(epsc[:], 1e-6), writes=[epsc])
    ht = [P.sb([128, 1024]) for _ in range(2)]; mt = [P.sb([128, 1024]) for _ in range(2)]; pt_ = [P.sb([128, 256]) for _ in range(2)]
    gt = [P.sb([128, 1024]) for _ in range(2)]; sg = P.sb([128, 1024])
    if rw:
        rowb = {}
        for n, d in rowd.items():
            rowb[n] = P.sb([128, 1024]); P.dma(rowb[n][:], d[:].partition_broadcast(128), reads=[d], writes=[rowb[n]])
        rt_ = P.sb([128, 1024]); kt_ = P.sb([128, 1024]); vt_ = P.sb([128, 1024]); xc = P.sb([128, 1024]); sq2 = P.sb([128, 1024])
        s16 = P.sb([128, 16]); q16 = P.sb([128, 16]); b16 = P.sb([128, 16]); eps2 = P.sb([128, 1])
        P.op("pool", lambda e: e.memset(eps2[:], 64e-5), writes=[eps2])
    mT = P.sb([128, 8, 128]); h1 = P.sb([128, 1024]); h1T = P.sb([128, 8, 128]); gate = P.sb([128, 1024]); pT = P.sb([128, 2, 128])
    tmp = P.sb([128, 1024]); h2 = [P.sb([128, 1024]) for _ in range(2)]
    sq = P.sb([128, 1024]); ss = P.sb([128, 1]); rs = P.sb([128, 1])
    ptr = [P.ps([128, 4, 128]) for _ in range(2)]
    py = [P.ps([128, 512]) for _ in range(2)]; pgt = [P.ps([128, 512]) for _ in range(2)]; ppp = [P.ps([128, 512]) for _ in range(2)]

    def transposes(src, nch, dstT):
        for g0 in range(0, nch, 4):
            p_ = ptr[(g0 // 4) % 2]
            n = min(4, nch - g0)
            for jj in range(n):
                j = g0 + jj
                P.op("pe", lambda e: e.transpose(p_[:, jj, :], src[:, j*128:(j+1)*128], idt[:]), reads=[src, idt], writes=[p_], inc=(jj == n - 1))
            if (g0 // 4) % 2 == 0:
                P.op("act", lambda e: e.copy(out=dstT[:, g0:g0+n, :], in_=p_[:, 0:n, :]), reads=[p_], writes=[(dstT, g0 // 4)])
            else:
                P.op("dve", lambda e: e.tensor_copy(out=dstT[:, g0:g0+n, :], in_=p_[:, 0:n, :]), reads=[p_], writes=[(dstT, g0 // 4)])

    for i in range(NT):
        b = i % 2
        P.dma(ht[b][:], h[i*128:(i+1)*128, :], reads=[h], writes=[ht[b]])
        P.dma(mt[b][:], m[i*128:(i+1)*128, :], reads=[m], writes=[mt[b]])
        P.dma(pt_[b][:], pin[i*128:(i+1)*128, :], reads=[pin], writes=[pt_[b]])
        P.dma(gt[b][:], gin[i*128:(i+1)*128, :], reads=[gin], writes=[gt[b]])
        P.op("act", lambda e: e.activation(out=sg[:], in_=gt[b][:], func=AF.Silu), reads=[gt[b]], writes=[sg])
        if rw:
            Y = mt[b]; sl_ = slice(i*128, (i+1)*128)
            P.dma(rt_[:], rin[sl_, :], reads=[rin], writes=[rt_]); P.dma(kt_[:], kin[sl_, :], reads=[kin], writes=[kt_]); P.dma(vt_[:], vin[sl_, :], reads=[vin], writes=[vt_])
            v3 = lambda t: t[:].rearrange("p (h f) -> p h f", f=64)
            P.op("dve", lambda e: e.reduce_sum(out=s16[:], in_=v3(Y), axis=AX.X), reads=[Y], writes=[s16])
            P.op("dve", lambda e: e.tensor_scalar(out=s16[:], in0=s16[:], scalar1=-1.0/64, scalar2=None, op0=ALU.mult), reads=[s16], writes=[s16])
            for hh in range(16):
                P.op("dve" if hh % 2 == 0 else "pool", lambda e: e.tensor_scalar(out=xc[:, hh*64:(hh+1)*64], in0=Y[:, hh*64:(hh+1)*64], scalar1=s16[:, hh:hh+1], scalar2=None, op0=ALU.add), reads=[Y, s16], writes=[(xc, hh)])
            P.op("act", lambda e: e.activation(out=sq2[:], in_=xc[:], func=AF.Square), reads=[xc], writes=[sq2])
            P.op("dve", lambda e: e.reduce_sum(out=q16[:], in_=v3(sq2), axis=AX.X), reads=[sq2], writes=[q16])
            P.op("act", lambda e: e.activation(out=q16[:], in_=q16[:], func=AF.Sqrt, scale=1.0/64, bias=eps2[:]), reads=[q16, eps2], writes=[q16])
            P.op("dve", lambda e: e.reciprocal(out=q16[:], in_=q16[:]), reads=[q16], writes=[q16])
            for hh in range(16):
                P.op("dve" if hh % 2 == 0 else "pool", lambda e: e.tensor_scalar(out=xc[:, hh*64:(hh+1)*64], in0=xc[:, hh*64:(hh+1)*64], scalar1=q16[:, hh:hh+1], scalar2=None, op0=ALU.mult), reads=[(xc, hh), q16], writes=[(xc, hh)])
            P.op("dve", lambda e: e.tensor_tensor(out=xc[:], in0=xc[:], in1=rowb["lnx_g"][:], op=ALU.mult), reads=[xc, rowb["lnx_g"]], writes=[xc])
            P.op("pool", lambda e: e.tensor_tensor(out=xc[:], in0=xc[:], in1=rowb["lnx_b"][:], op=ALU.add), reads=[xc, rowb["lnx_b"]], writes=[xc])
            P.op("dve", lambda e: e.tensor_tensor(out=sq2[:], in0=rt_[:], in1=kt_[:], op=ALU.mult), reads=[rt_, kt_], writes=[sq2])
            P.op("pool", lambda e: e.tensor_tensor(out=sq2[:], in0=sq2[:], in1=rowb["r_k"][:], op=ALU.mult), reads=[sq2, rowb["r_k"]], writes=[sq2])
            P.op("dve", lambda e: e.reduce_sum(out=b16[:], in_=v3(sq2), axis=AX.X), reads=[sq2], writes=[b16])
            for hh in range(16):
                P.op("dve", lambda e: e.scalar_tensor_tensor(out=xc[:, hh*64:(hh+1)*64], in0=vt_[:, hh*64:(hh+1)*64], scalar=b16[:, hh:hh+1], in1=xc[:, hh*64:(hh+1)*64], op0=ALU.mult, op1=ALU.add),
                     reads=[vt_, b16, xc], writes=[(xc, hh)])
            P.op("pool", lambda e: e.tensor_tensor(out=mt[b][:], in0=xc[:], in1=sg[:], op=ALU.mult), reads=[xc, sg], writes=[mt[b]])
        else:
            P.op("pool", lambda e: e.tensor_tensor(out=mt[b][:], in0=mt[b][:], in1=sg[:], op=ALU.mult), reads=[mt[b], sg], writes=[mt[b]])
        transposes(mt[b], 8, mT)
        for hf in range(2):
            for j in range(8):
                P.op("pe", lambda e: e.matmul(py[hf][:], lhsT=mT[:, j, :], rhs=wo[:, j, hf*512:(hf+1)*512], start=(j == 0), stop=(j == 7)),
                     reads=[mT, (wo, j)], writes=[py[hf]], inc=(j == 7))
            P.op("dve", lambda e: e.tensor_tensor(out=h1[:, hf*512:(hf+1)*512], in0=py[hf][:], in1=ht[b][:, hf*512:(hf+1)*512], op=ALU.add),
                 reads=[py[hf], ht[b]], writes=[(h1, hf)])
        transposes(h1, 8, h1T)
        for hf in range(2):
            for j in range(8):
                P.op("pe", lambda e: e.matmul(pgt[hf][:], lhsT=h1T[:, j, :], rhs=wg[:, j, hf*512:(hf+1)*512], start=(j == 0), stop=(j == 7)),
                     reads=[h1T, (wg, j)], writes=[pgt[hf]], inc=(j == 7))
            P.op("act", lambda e: e.activation(out=gate[:, hf*512:(hf+1)*512], in_=pgt[hf][:], func=AF.Sigmoid), reads=[pgt[hf]], writes=[(gate, hf)])
        transposes(pt_[b], 2, pT)
        for hf in range(2):
            for j in range(2):
                P.op("pe", lambda e: e.matmul(ppp[hf][:], lhsT=pT[:, j, :], rhs=wp[:, j, hf*512:(hf+1)*512], start=(j == 0), stop=(j == 1)),
                     reads=[pT, (wp, j)], writes=[ppp[hf]], inc=(j == 1))
            P.op("dve", lambda e: e.tensor_tensor(out=tmp[:, hf*512:(hf+1)*512], in0=ppp[hf][:], in1=gate[:, hf*512:(hf+1)*512], op=ALU.mult),
                 reads=[ppp[hf], (gate, hf)], writes=[(tmp, hf)])
        P.op("dve", lambda e: e.tensor_tensor(out=h2[b][:], in0=tmp[:], in1=h1[:], op=ALU.add), reads=[tmp, h1], writes=[h2[b]])
        if final:
            P.op("act", lambda e: e.activation(out=sq[:], in_=h2[b][:], func=AF.Square), reads=[h2[b]], writes=[sq])
            P.op("dve", lambda e: e.reduce_sum(out=ss[:], in_=sq[:], axis=AX.X), reads=[sq], writes=[ss])
            P.op("act", lambda e: e.activation(out=rs[:], in_=ss[:], func=AF.Sqrt, scale=1.0/1024, bias=epsc[:]), reads=[ss, epsc], writes=[rs])
            P.op("dve", lambda e: e.reciprocal(out=rs[:], in_=rs[:]), reads=[rs], writes=[rs])
            P.op("dve", lambda e: e.scalar_tensor_tensor(out=h2[b][:], in0=h2[b][:], scalar=rs[:, 0:1], in1=fgb[:], op0=ALU.mult, op1=ALU.mult),
                 reads=[h2[b], rs, fgb], writes=[h2[b]])
        P.dma(out[i*128:(i+1)*128, :], h2[b][:], reads=[h2[b]], writes=[(out, i)], q="pool")
    P.finish([out])
    return P


def sb_consts():
    j = np.arange(128)
    negtri = np.where(j[:, None] >= j[None, :], -1.0, 0.0).astype(np.float32)
    negones = -np.ones((128, 128), np.float32)
    t = np.arange(512)
    masks = np.stack([((m * 128 + j)[:, None] < t[None, :]).astype(np.float32) for m in range(4)], 0)
    return negtri, negones, masks

def build_sb(S=8192):
    P = Prog()
    NB = S // 128; NG = S // 512
    qd = P.dram("qb", [S, 128]); kd = P.dram("kb", [S, 128]); vd = P.dram("vb", [S, 128])
    ident_d = P.dram("ident", [128, 128]); nt_d = P.dram("negtri", [128, 128]); no_d = P.dram("negones", [128, 128]); mk_d = P.dram("masks", [4, 128, 512])
    outT = P.dram("obT", [128, S], kind="ExternalOutput")
    idt = P.sb([128, 128]); ntr = P.sb([128, 128]); non = P.sb([128, 128]); mk = P.sb([128, 4, 512])
    P.dma(idt[:], ident_d[:], reads=[ident_d], writes=[idt]); P.dma(ntr[:], nt_d[:], reads=[nt_d], writes=[ntr]); P.dma(non[:], no_d[:], reads=[no_d], writes=[non])
    for m in range(4):
        P.dma(mk[:, m, :], mk_d[m], reads=[mk_d], writes=[(mk, m)])
    one = P.sb([128, 1]); P.op("pool", lambda e: e.memset(one[:], 1.0), writes=[one])
    qT = P.sb([128, S]); kT = P.sb([128, S]); v = P.sb([128, NB, 128])
    ptr = [P.ps([128, 4, 128]) for _ in range(2)]
    ld = [P.sb([128, 4, 128]) for _ in range(4)]
    scale = 128 ** -0.5
    n = 0
    for src, dst, sc in ((qd, qT, scale), (kd, kT, 1.0)):
        for g in range(NB // 4):
            lb = ld[n % 4]; p_ = ptr[n % 2]
            P.dma(lb[:], src[g*512:(g+1)*512, :].rearrange("(j p) d -> p j d", p=128), reads=[src], writes=[lb])
            for jj in range(4):
                P.op("pe", lambda e: e.transpose(p_[:, jj, :], lb[:, jj, :], idt[:]), reads=[lb, idt], writes=[p_], inc=(jj == 3))
            if n % 2 == 0:
                P.op("act", lambda e: e.activation(out=dst[:, g*512:(g+1)*512], in_=p_[:].rearrange("p j t -> p (j t)"), func=AF.Copy, scale=sc), reads=[p_], writes=[(dst, g)])
            else:
                P.op("dve", lambda e: e.tensor_scalar(out=dst[:, g*512:(g+1)*512], in0=p_[:].rearrange("p j t -> p (j t)"), scalar1=sc, scalar2=None, op0=ALU.mult), reads=[p_], writes=[(dst, g)])
            n += 1
    for g in range(NB // 4):
        P.dma(v[:, g*4:(g+1)*4, :], vd[g*512:(g+1)*512, :].rearrange("(j p) d -> p j d", p=128), reads=[vd], writes=[(v, g)])
    psA = [P.ps([128, 512]) for _ in range(2)]; psB = [P.ps([128, 512]) for _ in range(2)]; psC = P.ps([128, 512]); psD = P.ps([128, 512])
    Eb = [P.sb([128, 512]) for _ in range(2)]; SPb = [P.sb([128, 512]) for _ in range(2)]; Wb = [P.sb([128, 512]) for _ in range(2)]
    nc_ = [P.sb([128, 512]) for _ in range(2)]; oT = [P.sb([128, 512]) for _ in range(2)]
    Xb = [P.sb([128, 512]) for _ in range(2)]; Tb = [P.sb([128, 512]) for _ in range(2)]
    n = 0
    for G in range(NG):
        qs = qT[:, G*512:(G+1)*512]
        kbs = list(range(4*G + 3, -1, -1))
        st = {"ci": 0}

        def stage1(idx, nn):
            kb = kbs[idx]; diag = kb >= 4*G; m = kb - 4*G
            A = psA[nn % 2]; E_ = Eb[nn % 2]; SP = SPb[nn % 2]
            ks = kT[:, kb*128:(kb+1)*128]
            P.op("pe", lambda e: e.matmul(A[:], lhsT=ks, rhs=qs, start=True, stop=True), reads=[(kT, kb // 4), (qT, G)], writes=[A])
            P.op("act", lambda e: e.activation(out=E_[:], in_=A[:], func=AF.Exp), reads=[A], writes=[E_])
            P.op("act", lambda e: e.activation(out=SP[:], in_=E_[:], func=AF.Ln, bias=one[:]), reads=[E_, one], writes=[SP])
            if diag:
                P.op("dve", lambda e: e.tensor_tensor(out=SP[:], in0=SP[:], in1=mk[:, m, :], op=ALU.mult), reads=[SP, (mk, m)], writes=[SP])

        def stage2(idx, nn):
            kb = kbs[idx]; first = (idx == 0); last = (kb == 0); diag = kb >= 4*G; m = kb - 4*G
            B = psB[nn % 2]; E_ = Eb[nn % 2]; SP = SPb[nn % 2]; W = Wb[nn % 2]; X = Xb[nn % 2]; T_ = Tb[nn % 2]
            ci = st["ci"]
            P.op("pe", lambda e: e.matmul(B[:], lhsT=ntr[:], rhs=SP[:], start=True, stop=True), reads=[ntr, SP], writes=[B])
            if first:
                P.op("act", lambda e: e.activation(out=X[:], in_=B[:], func=AF.Exp), reads=[B], writes=[X])
            else:
                P.op("dve", lambda e: e.tensor_tensor(out=T_[:], in0=B[:], in1=nc_[ci][:], op=ALU.add), reads=[B, nc_[ci]], writes=[T_])
                P.op("act", lambda e: e.activation(out=X[:], in_=T_[:], func=AF.Exp), reads=[T_], writes=[X])
            P.op("dve", lambda e: e.tensor_tensor(out=W[:], in0=E_[:], in1=X[:], op=ALU.mult), reads=[E_, X], writes=[W])
            if diag:
                P.op("dve", lambda e: e.tensor_tensor(out=W[:], in0=W[:], in1=mk[:, m, :], op=ALU.mult), reads=[W, (mk, m)], writes=[W])
            if not last:
                P.op("pe", lambda e: e.matmul(psC[:], lhsT=non[:], rhs=SP[:], start=True, stop=True), reads=[non, SP], writes=[psC])
                if first:
                    P.op("dve", lambda e: e.tensor_copy(out=nc_[0][:], in_=psC[:]), reads=[psC], writes=[nc_[0]]); st["ci"] = 0
                else:
                    P.op("dve", lambda e: e.tensor_tensor(out=nc_[1 - ci][:], in0=psC[:], in1=nc_[ci][:], op=ALU.add), reads=[psC, nc_[ci]], writes=[nc_[1 - ci]]); st["ci"] = 1 - ci
            P.op("pe", lambda e: e.matmul(psD[:], lhsT=v[:, kb, :], rhs=W[:], start=first, stop=last), reads=[(v, kb // 4), W], writes=[psD], inc=last)

        for idx in range(len(kbs) + 1):
            if idx < len(kbs):
                stage1(idx, n + idx)
            if idx >= 1:
                stage2(idx - 1, n + idx - 1)
        n += len(kbs)
        o_ = oT[G % 2]
        P.op("dve", lambda e: e.tensor_copy(out=o_[:], in_=psD[:]), reads=[psD], writes=[o_])
        P.dma(outT[:, G*512:(G+1)*512], o_[:], reads=[o_], writes=[(outT, G)], q="pool")
    P.finish([outT])
    return P

def sb_ref(q, k, v):
    f8 = np.float64
    q, k, v = q.astype(f8), k.astype(f8), v.astype(f8)
    S = q.shape[0]
    z = (q @ k.T) * 128 ** -0.5
    mask = np.arange(S)[None, :] < np.arange(S)[:, None]
    sp = np.where(mask, np.logaddexp(0, z), 0.0)
    rc = np.cumsum(sp[:, ::-1], axis=1)[:, ::-1]
    w = np.where(mask, np.exp(z - rc), 0.0)
    return w @ v


def ret_consts(head, S):
    f8 = np.float64
    gam = 1.0 - 2.0 ** (-5.0 - head)
    pos = np.arange(S, dtype=f8)
    inv = 10000.0 ** (-np.arange(64, dtype=f8) / 64)
    ang = pos[:, None] * inv[None, :]
    c, s = np.cos(ang), np.sin(ang)
    jj = (np.arange(S) % 128).astype(f8)
    ksc = (128 ** -0.5) * gam ** (-(jj + 1))
    i = np.arange(128)
    mask = (i[:, None] <= i[None, :]).astype(np.float32)
    gq = (gam ** (i + 1.0)).astype(np.float32).reshape(128, 1)
    gC = np.full((128, 1), gam ** 128.0, np.float32)
    return dict(cosq=c.astype(np.float32), sinq=s.astype(np.float32), cosk=(c * ksc[:, None]).astype(np.float32), sink=(s * ksc[:, None]).astype(np.float32),
                rmask=mask, gq=gq, gC=gC)

def build_ret(S=8192):
    P = Prog()
    NC = S // 128
    qd = P.dram("qa", [S, 128]); kd = P.dram("ka", [S, 128]); vd = P.dram("va", [S, 128])
    cq = P.dram("cosq", [S, 64]); sq_ = P.dram("sinq", [S, 64]); ck = P.dram("cosk", [S, 64]); sk = P.dram("sink", [S, 64])
    ident_d = P.dram("ident", [128, 128]); mk_d = P.dram("rmask", [128, 128]); gq_d = P.dram("gq", [128, 1]); gC_d = P.dram("gC", [128, 1]); gn_d = P.dram("gn", [1, 128])
    out = P.dram("oa", [S, 128], kind="ExternalOutput")
    idt = P.sb([128, 128]); mk = P.sb([128, 128]); gq = P.sb([128, 1]); gC = P.sb([128, 1]); gn = P.sb([128, 128])
    P.dma(idt[:], ident_d[:], reads=[ident_d], writes=[idt]); P.dma(mk[:], mk_d[:], reads=[mk_d], writes=[mk])
    P.dma(gq[:], gq_d[:], reads=[gq_d], writes=[gq]); P.dma(gC[:], gC_d[:], reads=[gC_d], writes=[gC])
    P.dma(gn[:], gn_d[:].partition_broadcast(128), reads=[gn_d], writes=[gn])
    eps = P.sb([128, 1]); P.op("pool", lambda e: e.memset(eps[:], 1e-5), writes=[eps])
    St = [P.sb([128, 128]) for _ in range(2)]
    P.op("pool", lambda e: e.memset(St[0][:], 0.0), writes=[St[0]])
    B2 = 2
    qt = [P.sb([128, 128]) for _ in range(B2)]; kt = [P.sb([128, 128]) for _ in range(B2)]; vt = [P.sb([128, 128]) for _ in range(B2)]
    cqt = [P.sb([128, 64]) for _ in range(B2)]; sqt = [P.sb([128, 64]) for _ in range(B2)]; ckt = [P.sb([128, 64]) for _ in range(B2)]; skt = [P.sb([128, 64]) for _ in range(B2)]
    qr = [P.sb([128, 128]) for _ in range(B2)]; kr = [P.sb([128, 128]) for _ in range(B2)]
    ta = P.sb([128, 64]); tb = P.sb([128, 64]); tc = P.sb([128, 64]); td = P.sb([128, 64])
    qkT = P.sb([128, 2, 128]); sTm = P.sb([128, 128]); os_ = P.sb([128, 128]); xc = P.sb([128, 128]); sqq = P.sb([128, 128])
    sm = P.sb([128, 1]); ss = P.sb([128, 1]); rs = P.sb([128, 1]); y = [P.sb([128, 128]) for _ in range(2)]
    ptr = P.ps([128, 2, 128]); ps_s = P.ps([128, 128]); ps_o = P.ps([128, 128]); ps_S = P.ps([128, 128])

    def rotary(eng, x, c, s, o, t1, t2):
        E = lambda f, r, w: P.op(eng, f, reads=r, writes=w)
        E(lambda e: e.tensor_tensor(out=t1[:], in0=x[:, 0:64], in1=c[:], op=ALU.mult), [x, c], [t1])
        E(lambda e: e.tensor_tensor(out=t2[:], in0=x[:, 64:128], in1=s[:], op=ALU.mult), [x, s], [t2])
        E(lambda e: e.tensor_tensor(out=o[:, 0:64], in0=t1[:], in1=t2[:], op=ALU.subtract), [t1, t2], [(o, 0)])
        E(lambda e: e.tensor_tensor(out=t1[:], in0=x[:, 0:64], in1=s[:], op=ALU.mult), [x, s], [t1])
        E(lambda e: e.tensor_tensor(out=t2[:], in0=x[:, 64:128], in1=c[:], op=ALU.mult), [x, c], [t2])
        E(lambda e: e.tensor_tensor(out=o[:, 64:128], in0=t1[:], in1=t2[:], op=ALU.add), [t1, t2], [(o, 1)])

    for c in range(NC):
        b = c % B2; sl = slice(c*128, (c+1)*128)
        for (dst, src) in ((qt[b], qd), (kt[b], kd), (vt[b], vd), (cqt[b], cq), (sqt[b], sq_), (ckt[b], ck), (skt[b], sk)):
            P.dma(dst[:], src[sl, :], reads=[src], writes=[dst])
        rotary("dve", qt[b], cqt[b], sqt[b], qr[b], ta, tb)
        rotary("pool", kt[b], ckt[b], skt[b], kr[b], tc, td)
        P.op("pe", lambda e: e.transpose(ptr[:, 0, :], qr[b][:], idt[:]), reads=[qr[b], idt], writes=[ptr], inc=False)
        P.op("pe", lambda e: e.transpose(ptr[:, 1, :], kr[b][:], idt[:]), reads=[kr[b], idt], writes=[ptr])
        P.op("dve", lambda e: e.tensor_copy(out=qkT[:], in_=ptr[:]), reads=[ptr], writes=[qkT])
        P.op("pe", lambda e: e.matmul(ps_s[:], lhsT=qkT[:, 1, :], rhs=qkT[:, 0, :], start=True, stop=True), reads=[qkT], writes=[ps_s])
        P.op("dve", lambda e: e.tensor_tensor(out=sTm[:], in0=ps_s[:], in1=mk[:], op=ALU.mult), reads=[ps_s, mk], writes=[sTm])
        Sc = St[c % 2]; Sn = St[(c + 1) % 2]
        P.op("pe", lambda e: e.matmul(ps_o[:], lhsT=sTm[:], rhs=vt[b][:], start=True, stop=False), reads=[sTm, vt[b]], writes=[ps_o], inc=False)
        P.op("pe", lambda e: e.matmul(ps_o[:], lhsT=qkT[:, 0, :], rhs=Sc[:], start=False, stop=True), reads=[qkT, Sc], writes=[ps_o])
        P.op("act", lambda e: e.activation(out=os_[:], in_=ps_o[:], func=AF.Copy, scale=gq[:, 0:1]), reads=[ps_o, gq], writes=[os_])
        P.op("pe", lambda e: e.matmul(ps_S[:], lhsT=kr[b][:], rhs=vt[b][:], start=True, stop=False), reads=[kr[b], vt[b]], writes=[ps_S], inc=False)
        P.op("pe", lambda e: e.matmul(ps_S[:], lhsT=idt[:], rhs=Sc[:], start=False, stop=True), reads=[idt, Sc], writes=[ps_S])
        P.op("act", lambda e: e.activation(out=Sn[:], in_=ps_S[:], func=AF.Copy, scale=gC[:, 0:1]), reads=[ps_S, gC], writes=[Sn])
        P.op("dve", lambda e: e.reduce_sum(out=sm[:], in_=os_[:], axis=AX.X), reads=[os_], writes=[sm])
        P.op("dve", lambda e: e.tensor_scalar(out=sm[:], in0=sm[:], scalar1=-1.0/128, scalar2=None, op0=ALU.mult), reads=[sm], writes=[sm])
        P.op("dve", lambda e: e.tensor_scalar(out=xc[:], in0=os_[:], scalar1=sm[:, 0:1], scalar2=None, op0=ALU.add), reads=[os_, sm], writes=[xc])
        P.op("act", lambda e: e.activation(out=sqq[:], in_=xc[:], func=AF.Square), reads=[xc], writes=[sqq])
        P.op("dve", lambda e: e.reduce_sum(out=ss[:], in_=sqq[:], axis=AX.X), reads=[sqq], writes=[ss])
        P.op("act", lambda e: e.activation(out=rs[:], in_=ss[:], func=AF.Sqrt, scale=1.0/128, bias=eps[:]), reads=[ss, eps], writes=[rs])
        P.op("dve", lambda e: e.reciprocal(out=rs[:], in_=rs[:]), reads=[rs], writes=[rs])
        yy = y[c % 2]
        P.op("dve", lambda e: e.scalar_tensor_tensor(out=yy[:], in0=xc[:], scalar=rs[:, 0:1], in1=gn[:], op0=ALU.mult, op1=ALU.mult), reads=[xc, rs, gn], writes=[yy])
        P.dma(out[sl, :], yy[:], reads=[yy], writes=[(out, c)], q="pool")
    P.finish([out])
    return P

def ret_ref(q, k, v, head, gn):
    f8 = np.float64
    q, k, v = q.astype(f8), k.astype(f8), v.astype(f8)
    S = q.shape[0]
    gam = 1.0 - 2.0 ** (-5.0 - head)
    pos = np.arange(S, dtype=f8); inv = 10000.0 ** (-np.arange(64, dtype=f8) / 64); ang = pos[:, None] * inv[None, :]
    c, s = np.cos(ang), np.sin(ang)
    rot = lambda x: np.concatenate([x[:, :64] * c - x[:, 64:] * s, x[:, :64] * s + x[:, 64:] * c], -1)
    q = rot(q); k = rot(k) * 128 ** -0.5
    out = np.zeros((S, 128)); St = np.zeros((128, 128))
    i = np.arange(128)
    D = np.where(i[:, None] >= i[None, :], gam ** np.maximum(i[:, None] - i[None, :], 0).astype(f8), 0.0)
    for n in range(S // 128):
        sl = slice(n*128, (n+1)*128)
        out[sl] = ((q[sl] @ k[sl].T) * D) @ v[sl] + (q[sl] * (gam ** (i + 1.0))[:, None]) @ St
        St = St * gam ** 128 + (k[sl] * (gam ** (127.0 - i))[:, None]).T @ v[sl]
    mu = out.mean(-1, keepdims=True); var = ((out - mu) ** 2).mean(-1, keepdims=True)
    return (out - mu) / np.sqrt(var + 1e-5) * gn


def build_o1(NT=16, vres=False):
    P = Prog()
    T = NT * 128
    h = P.dram("h", [T, 1024]); hprev = P.dram("hprev", [1, 1024]); gain = P.dram("gain", [128, 8]); mu = P.dram("mu", [128, 48])
    w_in = P.dram("w_in", [4, 1024, 1024])
    w1 = P.dram("w1", [1024, 64]); w2 = P.dram("w2", [64, 1024]); a1 = P.dram("a1", [1024, 64]); a2 = P.dram("a2", [64, 1024])
    v1 = P.dram("v1", [1024, 32]); v2 = P.dram("v2", [32, 1024]); vfirst = P.dram("vfirst", [T, 1024])
    rows = {n: P.dram(n, [1, 1024]) for n in ["w0", "a0", "v0", "k_k", "k_a"]}
    ident_d = P.dram("ident", [128, 128])
    outs = {n: P.dram("o_" + n, [T, 1024], kind="ExternalOutput") for n in ["r", "lw", "k", "v", "kk", "b", "g"]}
    idt = P.sb([128, 128]); P.dma(idt[:], ident_d[:], reads=[ident_d], writes=[idt])
    g = P.sb([128, 8]); P.dma(g[:], gain[:], reads=[gain], writes=[g])
    mus = P.sb([128, 48]); P.dma(mus[:], mu[:], reads=[mu], writes=[mus])
    omu = P.sb([128, 48]); P.op("dve", lambda e: e.tensor_scalar(out=omu[:], in0=mus[:], scalar1=-1.0, scalar2=1.0, op0=ALU.mult, op1=ALU.add), reads=[mus], writes=[omu])
    rb = {}
    for n, d in rows.items():
        if n == "v0" and not vres:
            continue
        rb[n] = P.sb([128, 1024]); P.dma(rb[n][:], d[:].partition_broadcast(128), reads=[d], writes=[rb[n]])
    w1s = P.sb([128, 8, 64]); a1s = P.sb([128, 8, 64]); w2s = P.sb([64, 1024]); a2s = P.sb([64, 1024])
    P.dma(w1s[:], w1[:].rearrange("(j p) c -> p j c", p=128), reads=[w1], writes=[w1s]); P.dma(a1s[:], a1[:].rearrange("(j p) c -> p j c", p=128), reads=[a1], writes=[a1s])
    P.dma(w2s[:], w2[:], reads=[w2], writes=[w2s]); P.dma(a2s[:], a2[:], reads=[a2], writes=[a2s])
    if vres:
        v1s = P.sb([128, 8, 32]); v2s = P.sb([32, 1024])
        P.dma(v1s[:], v1[:].rearrange("(j p) c -> p j c", p=128), reads=[v1], writes=[v1s]); P.dma(v2s[:], v2[:], reads=[v2], writes=[v2s])
    epsc = P.sb([128, 1]); P.op("pool", lambda e: e.memset(epsc[:], 1e-6), writes=[epsc])
    one = P.sb([128, 1]); P.op("pool", lambda e: e.memset(one[:], 1.0), writes=[one])
    mhalf = P.sb([128, 1]); P.op("pool", lambda e: e.memset(mhalf[:], -0.5), writes=[mhalf])
    ht = [P.sb([128, 1024]) for _ in range(2)]; sq = P.sb([128, 1024]); hn = P.sb([128, 1024]); ss = P.sb([128, 1]); rs = P.sb([128, 1])
    hnT = [P.sb([128, 8, 132]) for _ in range(2)]
    xs = [P.sb([128, 8, 128]) for _ in range(2)]; tmpx = [P.sb([128, 8, 128]) for _ in range(2)]
    wbuf = [P.sb([128, 8, 512]) for _ in range(2)]
    ptr = [P.ps([128, 4, 128]) for _ in range(2)]; pacc = [P.ps([128, 512]) for _ in range(2)]; pl1 = P.ps([128, 128]); pl2 = [P.ps([128, 512]) for _ in range(2)]
    l1T = P.sb([64, 128])
    A_ = P.sb([128, 1024]); kraw = P.sb([128, 1024]); kkr = P.sb([128, 1024]); t1 = P.sb([128, 1024]); t2 = P.sb([128, 1024])
    ssk = P.sb([128, 16]); rn = P.sb([128, 16])
    ob = {n: [P.sb([128, 1024]) for _ in range(2)] for n in ["r", "v", "g"]}
    for n in ["lw", "k", "kk", "b"]:
        _t = P.sb([128, 1024]); ob[n] = [_t, _t]
    vf = P.sb([128, 1024]); sgv = P.sb([128, 1024])
    wn = [0]

    def norm_T(src, dst, cols):
        n = cols.stop - cols.start
        P.op("act", lambda e: e.activation(out=sq[:], in_=src[:], func=AF.Square), reads=[src], writes=[sq])
        P.op("dve", lambda e: e.reduce_sum(out=ss[:], in_=sq[:], axis=AX.X), reads=[sq], writes=[ss])
        P.op("act", lambda e: e.activation(out=rs[:], in_=ss[:], func=AF.Sqrt, scale=1.0/1024, bias=epsc[:]), reads=[ss, epsc], writes=[rs])
        P.op("dve", lambda e: e.reciprocal(out=rs[:], in_=rs[:]), reads=[rs], writes=[rs])
        P.op("dve", lambda e: e.tensor_scalar(out=hn[:], in0=src[:], scalar1=rs[:, 0:1], scalar2=None, op0=ALU.mult), reads=[src, rs], writes=[hn])
        for half in range(2):
            p_ = ptr[half]
            for jj in range(4):
                j = half*4 + jj
                P.op("pe", lambda e: e.transpose(p_[:, jj, :], hn[:, j*128:(j+1)*128], idt[:]), reads=[hn, idt], writes=[p_], inc=(jj == 3))
            for jj in range(4):
                j = half*4 + jj
                if half == 0:
                    P.op("act", lambda e: e.activation(out=dst[:, j, cols], in_=p_[:, jj, 0:n], func=AF.Copy, scale=g[:, j:j+1]), reads=[p_, g], writes=[(dst, "c")])
                else:
                    P.op("dve", lambda e: e.tensor_scalar(out=dst[:, j, cols], in0=p_[:, jj, 0:n], scalar1=g[:, j:j+1], scalar2=None, op0=ALU.mult), reads=[p_, g], writes=[(dst, "c")])

    P.op("pool", lambda e: e.memset(ht[1][:], 0.0), writes=[ht[1]])
    P.dma(ht[1][0:1, :], hprev[:], reads=[hprev], writes=[ht[1]])
    norm_T(ht[1], hnT[0], slice(0, 1))
    for i in range(NT):
        b = i % 2; HT = hnT[b]; sl = slice(i*128, (i+1)*128)
        P.dma(ht[b][:], h[sl, :], reads=[h], writes=[ht[b]])
        if vres:
            P.dma(vf[:], vfirst[sl, :], reads=[vfirst], writes=[vf])
        norm_T(ht[b], HT, slice(1, 129))
        P.op("pool", lambda e: e.tensor_copy(out=hnT[1 - b][:, :, 0:1], in_=HT[:, :, 128:129]), reads=[HT], writes=[(hnT[1 - b], "p")])
        xi = 0
        for p in (5, 1, 4, 2, 0, 3):
            X = xs[xi % 2]; TX = tmpx[xi % 2]; xi += 1
            for j in range(8):
                P.op("pool", lambda e: e.tensor_scalar(out=TX[:, j, :], in0=HT[:, j, 0:128], scalar1=mus[:, p*8+j:p*8+j+1], scalar2=None, op0=ALU.mult), reads=[HT, mus], writes=[(TX, j)])
                P.op("dve", lambda e: e.scalar_tensor_tensor(out=X[:, j, :], in0=HT[:, j, 1:129], scalar=omu[:, p*8+j:p*8+j+1], in1=TX[:, j, :], op0=ALU.mult, op1=ALU.add),
                     reads=[HT, omu, (TX, j)], writes=[(X, j)])
            lora = None
            if p == 5: lora = (a1s, a2s, 64, "a")
            if p == 4: lora = (w1s, w2s, 64, "w")
            if p == 2 and vres: lora = (v1s, v2s, 32, "v")
            if p < 4:
                for half in range(2):
                    wb = wbuf[wn[0] % 2]; wn[0] += 1
                    for j in range(8):
                        P.dma(wb[:, j, :], w_in[p, j*128:(j+1)*128, half*512:(half+1)*512], reads=[w_in], writes=[(wb, j)])
                    pa = pacc[half]
                    for j in range(8):
                        P.op("pe", lambda e: e.matmul(pa[:], lhsT=X[:, j, :], rhs=wb[:, j, :], start=(j == 0), stop=(j == 7)), reads=[(X, j), (wb, j)], writes=[pa], inc=(j == 7))
                    hsl = slice(half*512, (half+1)*512)
                    if p == 0:
                        P.op("act", lambda e: e.copy(out=ob["r"][b][:, hsl], in_=pa[:]), reads=[pa], writes=[(ob["r"][b], half)])
                    elif p == 3:
                        P.op("act", lambda e: e.copy(out=ob["g"][b][:, hsl], in_=pa[:]), reads=[pa], writes=[(ob["g"][b], half)])
                    elif p == 1:
                        P.op("act", lambda e: e.copy(out=kraw[:, hsl], in_=pa[:]), reads=[pa], writes=[(kraw, half)])
                    elif p == 2:
                        P.op("act", lambda e: e.copy(out=ob["v"][b][:, hsl], in_=pa[:]), reads=[pa], writes=[(ob["v"][b], half)])
            if lora is not None:
                l1w, l2w, R, kind = lora
                for j in range(8):
                    P.op("pe", lambda e: e.matmul(pl1[0:R, :], lhsT=l1w[:, j, :], rhs=X[:, j, :], start=(j == 0), stop=(j == 7)), reads=[l1w, (X, j)], writes=[pl1], inc=(j == 7))
                if kind == "w":
                    P.op("act", lambda e: e.activation(out=l1T[0:R, :], in_=pl1[0:R, :], func=AF.Tanh), reads=[pl1], writes=[l1T])
                else:
                    P.op("act", lambda e: e.copy(out=l1T[0:R, :], in_=pl1[0:R, :]), reads=[pl1], writes=[l1T])
                for half in range(2):
                    hsl = slice(half*512, (half+1)*512)
                    P.op("pe", lambda e: e.matmul(pl2[half][:], lhsT=l1T[0:R, :], rhs=l2w[:, hsl], start=True, stop=True), reads=[l1T, l2w], writes=[pl2[half]])
                    if kind == "a":
                        P.op("dve", lambda e: e.tensor_tensor(out=t1[:, hsl], in0=pl2[half][:], in1=rb["a0"][:, hsl], op=ALU.add), reads=[pl2[half], rb["a0"]], writes=[(t1, half)])
                        P.op("act", lambda e: e.activation(out=A_[:, hsl], in_=t1[:, hsl], func=AF.Sigmoid), reads=[(t1, half)], writes=[(A_, half)])
                    elif kind == "w":
                        P.op("dve", lambda e: e.tensor_tensor(out=t1[:, hsl], in0=pl2[half][:], in1=rb["w0"][:, hsl], op=ALU.add), reads=[pl2[half], rb["w0"]], writes=[(t1, half)])
                        P.op("act", lambda e: e.activation(out=t2[:, hsl], in_=t1[:, hsl], func=AF.Exp, scale=-1.0), reads=[(t1, half)], writes=[(t2, half)])
                        P.op("act", lambda e: e.activation(out=t1[:, hsl], in_=t2[:, hsl], func=AF.Ln, bias=one[:]), reads=[(t2, half), one], writes=[(t1, half)])
                        P.op("act", lambda e: e.activation(out=t2[:, hsl], in_=t1[:, hsl], func=AF.Exp, scale=-1.0, bias=mhalf[:]), reads=[(t1, half), mhalf], writes=[(t2, half)])
                        P.op("dve", lambda e: e.tensor_scalar(out=ob["lw"][b][:, hsl], in0=t2[:, hsl], scalar1=-1.0, scalar2=None, op0=ALU.mult), reads=[(t2, half)], writes=[(ob["lw"][b], half)])
                    else:
                        P.op("dve", lambda e: e.tensor_tensor(out=t1[:, hsl], in0=pl2[half][:], in1=rb["v0"][:, hsl], op=ALU.add), reads=[pl2[half], rb["v0"]], writes=[(t1, half)])
                        P.op("act", lambda e: e.activation(out=sgv[:, hsl], in_=t1[:, hsl], func=AF.Sigmoid), reads=[(t1, half)], writes=[(sgv, half)])
            if p == 1:
                KK = ob["kk"][b]
                P.op("dve", lambda e: e.tensor_tensor(out=kkr[:], in0=kraw[:], in1=rb["k_k"][:], op=ALU.mult), reads=[kraw, rb["k_k"]], writes=[kkr])
                P.op("pool", lambda e: e.tensor_tensor(out=t1[:], in0=kkr[:], in1=kkr[:], op=ALU.mult), reads=[kkr], writes=[t1])
                P.op("dve", lambda e: e.reduce_sum(out=ssk[:], in_=t1[:].rearrange("p (h f) -> p h f", f=64), axis=AX.X), reads=[t1], writes=[ssk])
                P.op("act", lambda e: e.activation(out=rn[:], in_=ssk[:], func=AF.Sqrt), reads=[ssk], writes=[rn])
                P.op("dve", lambda e: e.tensor_scalar(out=rn[:], in0=rn[:], scalar1=1e-12, scalar2=None, op0=ALU.max), reads=[rn], writes=[rn])
                P.op("dve", lambda e: e.reciprocal(out=rn[:], in_=rn[:]), reads=[rn], writes=[rn])
                for hh in range(16):
                    eng = "dve" if hh % 2 == 0 else "pool"
                    P.op(eng, lambda e: e.tensor_scalar(out=KK[:, hh*64:(hh+1)*64], in0=kkr[:, hh*64:(hh+1)*64], scalar1=rn[:, hh:hh+1], scalar2=None, op0=ALU.mult), reads=[kkr, rn], writes=[(KK, hh)])
                P.op("dve", lambda e: e.scalar_tensor_tensor(out=t2[:], in0=A_[:], scalar=-1.0, in1=rb["k_a"][:], op0=ALU.add, op1=ALU.mult), reads=[A_, rb["k_a"]], writes=[t2])
                P.op("dve", lambda e: e.scalar_tensor_tensor(out=ob["k"][b][:], in0=t2[:], scalar=1.0, in1=kraw[:], op0=ALU.add, op1=ALU.mult), reads=[t2, kraw], writes=[ob["k"][b]])
                P.op("pool", lambda e: e.tensor_tensor(out=ob["b"][b][:], in0=KK[:], in1=A_[:], op=ALU.mult), reads=[KK, A_], writes=[ob["b"][b]])
                for n in ("k", "kk", "b"):
                    P.dma(outs[n][sl, :], ob[n][b][:], reads=[ob[n][b]], writes=[(outs[n], i)], q="pool")
            if p == 4:
                P.dma(outs["lw"][sl, :], ob["lw"][b][:], reads=[ob["lw"][b]], writes=[(outs["lw"], i)], q="pool")
            if p == 2:
                if vres:
                    V = ob["v"][b]
                    P.op("pool", lambda e: e.tensor_tensor(out=t1[:], in0=vf[:], in1=V[:], op=ALU.subtract), reads=[vf, V], writes=[t1])
                    P.op("dve", lambda e: e.tensor_tensor(out=t1[:], in0=t1[:], in1=sgv[:], op=ALU.mult), reads=[t1, sgv], writes=[t1])
                    P.op("dve", lambda e: e.tensor_tensor(out=V[:], in0=V[:], in1=t1[:], op=ALU.add), reads=[V, t1], writes=[V])
                P.dma(outs["v"][sl, :], ob["v"][b][:], reads=[ob["v"][b]], writes=[(outs["v"], i)], q="pool")
            if p == 0:
                P.dma(outs["r"][sl, :], ob["r"][b][:], reads=[ob["r"][b]], writes=[(outs["r"], i)], q="pool")
            if p == 3:
                P.dma(outs["g"][sl, :], ob["g"][b][:], reads=[ob["g"][b]], writes=[(outs["g"], i)], q="pool")
    P.finish(list(outs.values()))
    return P

def o1_inputs(inp, o, i_layer, hsh, hprev, vfirst_sh, vres):
    f32 = np.float32; A = lambda a: np.ascontiguousarray(a, dtype=f32)
    d = dict(h=hsh, hprev=hprev, gain=A(inp["norm_gain"][i_layer].reshape(8, 128).T),
             mu=A(inp["odd_mu"][o].reshape(6, 8, 128).transpose(2, 0, 1).reshape(128, 48)),
             w_in=A(inp["odd_w_in"][o]), w1=A(inp["rwkv_w1"][o]), w2=A(inp["rwkv_w2"][o]), a1=A(inp["rwkv_a1"][o]), a2=A(inp["rwkv_a2"][o]),
             w0=A(inp["rwkv_w0"][o]).reshape(1, 1024), a0=A(inp["rwkv_a0"][o]).reshape(1, 1024), k_k=A(inp["rwkv_k_k"][o]).reshape(1, 1024), k_a=A(inp["rwkv_k_a"][o]).reshape(1, 1024),
             ident=np.eye(128, dtype=f32))
    if vres:
        d.update(v1=A(inp["rwkv_v1"][o - 1]), v2=A(inp["rwkv_v2"][o - 1]), v0=A(inp["rwkv_v0"][o - 1]).reshape(1, 1024), vfirst=vfirst_sh)
    else:
        d.update(v1=np.zeros((1024, 32), f32), v2=np.zeros((32, 1024), f32), v0=np.zeros((1, 1024), f32), vfirst=np.zeros_like(hsh))
    return d


def o2_consts():
    i = np.arange(128)
    f = lambda m: np.ascontiguousarray(m.astype(np.float32))
    tri_incl = f(i[:, None] <= i[None, :])
    tri_excl = f(i[:, None] < i[None, :])
    tri_up = f(i[:, None] > i[None, :])
    ones = np.ones((128, 128), np.float32)
    slT = f(i[:, None] < i[None, :])
    ilT = f(i[:, None] <= i[None, :])
    sl = f(i[None, :] < i[:, None])
    m1 = np.concatenate([-slT, ilT], 1); m2 = np.concatenate([slT, ilT], 1)
    return dict(tri_incl=tri_incl, tri_excl=tri_excl, tri_up=tri_up, ones=ones,
                maskM1=np.concatenate([m1, m1], 1), maskM2=np.concatenate([m2, m2], 1), negsl4=np.concatenate([-sl] * 4, 1),
                identrep=np.concatenate([np.eye(64, dtype=np.float32)] * 4, 1), ident4=np.concatenate([np.eye(128, dtype=np.float32)] * 4, 1),
                ident=np.eye(128, dtype=np.float32))

def build_o2_body(P, din, yout, cdram, ident_d, S):
    NCH = S // 128
    names = ["r", "lw", "k", "v", "kk", "b"]
    cn = {}
    cshape = dict(tri_incl=[128, 128], tri_excl=[128, 128], tri_up=[128, 128], ones=[128, 128], maskM1=[128, 512], maskM2=[128, 512], negsl4=[128, 512],
                  identrep=[64, 256], ident4=[128, 512], ident=[128, 128])
    for n_, shp in cshape.items():
        d = ident_d if n_ == "ident" else cdram[n_]
        t = P.sb(shp); P.dma(t[:], d[:], reads=[d], writes=[t]); cn[n_] = t
    idt = cn["ident"]
    bank = [P.ps([128, 512]) for _ in range(8)]
    inb = [{n: P.sb([128, 256]) for n in names} for _ in range(2)]
    ex = {n: P.sb([128, 256]) for n in ["pos", "neg", "prev", "hat"]}
    etot = P.sb([64, 256]); diagG = P.sb([64, 256])
    sc = {n: P.sb([128, 256]) for n in ["rt", "kkt", "bt", "kt", "bh", "kh"]}
    KR = P.sb([64, 4, 2, 128]); BK = P.sb([64, 4, 2, 128])
    M1s = P.sb([128, 4, 256]); M2s = P.sb([128, 4, 256]); Xs = P.sb([128, 4, 128])
    Zp = [P.sb([128, 4, 128]) for _ in range(2)]; Xp = [P.sb([128, 4, 128]) for _ in range(2)]; Tt = [P.sb([128, 4, 128]) for _ in range(2)]
    nr1 = P.sb([128, 256]); U = P.sb([128, 256]); Y = [P.sb([128, 256]) for _ in range(2)]
    ST = [P.sb([64, 256]) for _ in range(2)]
    P.op("pool", lambda e: e.memset(ST[0][:], 0.0), writes=[ST[0]])
    hs = lambda h: slice(h*64, (h+1)*64)
    c4 = lambda h: slice(h*128, (h+1)*128)
    for c in range(NCH):
        ib = inb[c % 2]; sl = slice(c*128, (c+1)*128)
        for n_ in names:
            P.dma(ib[n_][:], din[n_][sl, :], reads=[din[n_]], writes=[ib[n_]])
        P.op("pe", lambda e: e.matmul(bank[0][:, 0:256], lhsT=cn["tri_incl"][:], rhs=ib["lw"][:], start=True, stop=True), reads=[cn["tri_incl"], ib["lw"]], writes=[bank[0]], inc=False)
        P.op("pe", lambda e: e.matmul(bank[0][:, 256:512], lhsT=cn["tri_excl"][:], rhs=ib["lw"][:], start=True, stop=True), reads=[cn["tri_excl"], ib["lw"]], writes=[bank[0]])
        P.op("pe", lambda e: e.matmul(bank[1][:, 0:256], lhsT=cn["tri_up"][:], rhs=ib["lw"][:], start=True, stop=True), reads=[cn["tri_up"], ib["lw"]], writes=[bank[1]], inc=False)
        P.op("pe", lambda e: e.matmul(bank[1][:, 256:512], lhsT=cn["ones"][:], rhs=ib["lw"][:], start=True, stop=True), reads=[cn["ones"], ib["lw"]], writes=[bank[1]])
        P.op("act", lambda e: e.activation(out=ex["pos"][:], in_=bank[0][:, 0:256], func=AF.Exp), reads=[bank[0]], writes=[ex["pos"]])
        P.op("act", lambda e: e.activation(out=ex["neg"][:], in_=bank[0][:, 0:256], func=AF.Exp, scale=-1.0), reads=[bank[0]], writes=[ex["neg"]])
        P.op("act", lambda e: e.activation(out=ex["prev"][:], in_=bank[0][:, 256:512], func=AF.Exp), reads=[bank[0]], writes=[ex["prev"]])
        P.op("act", lambda e: e.activation(out=ex["hat"][:], in_=bank[1][:, 0:256], func=AF.Exp), reads=[bank[1]], writes=[ex["hat"]])
        P.op("act", lambda e: e.activation(out=etot[:], in_=bank[1][0:64, 256:512], func=AF.Exp), reads=[bank[1]], writes=[etot])
        P.op("pool", lambda e: e.tensor_tensor(out=diagG[:], in0=etot[:], in1=cn["identrep"][:], op=ALU.mult), reads=[etot, cn["identrep"]], writes=[diagG])
        for (o_, a_, e_, eng) in (("rt", "r", "pos", "dve"), ("kkt", "kk", "prev", "dve"), ("bt", "b", "neg", "dve"), ("kt", "k", "neg", "dve"), ("bh", "b", "hat", "pool"), ("kh", "k", "hat", "pool")):
            P.op(eng, lambda e: e.tensor_tensor(out=sc[o_][:], in0=ib[a_][:], in1=ex[e_][:], op=ALU.mult), reads=[ib[a_], ex[e_]], writes=[sc[o_]])
        for (dst, pair, b0) in ((KR, ("kkt", "rt"), 2), (BK, ("bt", "kt"), 4)):
            for hp in range(2):
                bk_ = bank[b0 + hp]
                for hh in range(2):
                    h = hp*2 + hh
                    for a in range(2):
                        P.op("pe", lambda e: e.transpose(bk_[0:64, (hh*2+a)*128:(hh*2+a+1)*128], sc[pair[a]][:, hs(h)], idt[:]), reads=[sc[pair[a]], idt], writes=[bk_], inc=(hh == 1 and a == 1))
                eng = "act" if hp == 0 else "dve"
                dv = dst[:, hp*2:(hp+1)*2].rearrange("p h a t -> p (h a t)")
                if eng == "act":
                    P.op("act", lambda e: e.copy(out=dv, in_=bk_[0:64, :]), reads=[bk_], writes=[(dst, hp)])
                else:
                    P.op("dve", lambda e: e.tensor_copy(out=dv, in_=bk_[0:64, :]), reads=[bk_], writes=[(dst, hp)])
        for hp in range(2):
            for hh in range(2):
                h = hp*2 + hh
                P.op("pe", lambda e: e.matmul(bank[hp][:, hh*256:(hh+1)*256], lhsT=BK[:, h, 0, :], rhs=KR[:, h].rearrange("p a t -> p (a t)"), start=True, stop=True), reads=[(BK, hp), (KR, hp)], writes=[bank[hp]], inc=(hh == 1))
            P.op("dve", lambda e: e.tensor_tensor(out=M1s[:, hp*2:(hp+1)*2].rearrange("p h c -> p (h c)"), in0=bank[hp][:], in1=cn["maskM1"][:], op=ALU.mult), reads=[bank[hp], cn["maskM1"]], writes=[(M1s, hp)])
            for hh in range(2):
                h = hp*2 + hh
                P.op("pe", lambda e: e.matmul(bank[6+hp][:, hh*256:(hh+1)*256], lhsT=BK[:, h, 1, :], rhs=KR[:, h].rearrange("p a t -> p (a t)"), start=True, stop=True), reads=[(BK, hp), (KR, hp)], writes=[bank[6+hp]], inc=(hh == 1))
            P.op("dve", lambda e: e.tensor_tensor(out=M2s[:, hp*2:(hp+1)*2].rearrange("p h c -> p (h c)"), in0=bank[6+hp][:], in1=cn["maskM2"][:], op=ALU.mult), reads=[bank[6+hp], cn["maskM2"]], writes=[(M2s, hp)])
        for h in range(4):
            P.op("pe", lambda e: e.matmul(bank[2][:, c4(h)], lhsT=KR[:, h, 0, :], rhs=BK[:, h, 0, :], start=True, stop=True), reads=[KR, BK], writes=[bank[2]], inc=(h == 3))
        P.op("dve", lambda e: e.tensor_tensor(out=Xp[0][:].rearrange("p h t -> p (h t)"), in0=bank[2][:], in1=cn["negsl4"][:], op=ALU.mult), reads=[bank[2], cn["negsl4"]], writes=[Xp[0]])
        P.op("dve", lambda e: e.tensor_copy(out=Zp[0][:], in_=M1s[:, :, 0:128]), reads=[M1s], writes=[Zp[0]])
        P.op("dve", lambda e: e.tensor_tensor(out=Tt[0][:].rearrange("p h t -> p (h t)"), in0=Zp[0][:].rearrange("p h t -> p (h t)"), in1=cn["ident4"][:], op=ALU.add), reads=[Zp[0], cn["ident4"]], writes=[Tt[0]])
        cur = 0
        for it in range(6):
            nx = 1 - cur
            for h in range(4):
                P.op("pe", lambda e: e.matmul(bank[3][:, c4(h)], lhsT=Zp[cur][:, h, :], rhs=Xp[cur][:, h, :], start=True, stop=True), reads=[Zp[cur], Xp[cur]], writes=[bank[3]], inc=(h == 3))
            P.op("act", lambda e: e.copy(out=Xp[nx][:].rearrange("p h t -> p (h t)"), in_=bank[3][:]), reads=[bank[3]], writes=[Xp[nx]])
            if it < 5:
                for h in range(4):
                    P.op("pe", lambda e: e.matmul(bank[4][:, c4(h)], lhsT=Xp[cur][:, h, :], rhs=Zp[cur][:, h, :], start=True, stop=True), reads=[Zp[cur], Xp[cur]], writes=[bank[4]], inc=(h == 3))
                P.op("dve", lambda e: e.tensor_copy(out=Zp[nx][:].rearrange("p h t -> p (h t)"), in_=bank[4][:]), reads=[bank[4]], writes=[Zp[nx]])
            for h in range(4):
                P.op("pe", lambda e: e.matmul(bank[5][:, c4(h)], lhsT=idt[:], rhs=Tt[cur][:, h, :], start=True, stop=False), reads=[idt, Tt[cur]], writes=[bank[5]], inc=False)
                P.op("pe", lambda e: e.matmul(bank[5][:, c4(h)], lhsT=Xp[nx][:, h, :], rhs=Tt[cur][:, h, :], start=False, stop=True), reads=[Xp[nx], Tt[cur]], writes=[bank[5]], inc=(h == 3))
            P.op("dve", lambda e: e.tensor_copy(out=Tt[nx][:].rearrange("p h t -> p (h t)"), in_=bank[5][:]), reads=[bank[5]], writes=[Tt[nx]])
            cur = nx
        TT = Tt[cur]
        Sc = ST[c % 2]; Sn = ST[(c + 1) % 2]; V = ib["v"]
        for h in range(4):
            P.op("pe", lambda e: e.matmul(bank[6][:, hs(h)], lhsT=KR[:, h, 0, :], rhs=Sc[:, hs(h)], start=True, stop=False), reads=[KR, Sc], writes=[bank[6]], inc=False)
            P.op("pe", lambda e: e.matmul(bank[6][:, hs(h)], lhsT=M2s[:, h, 0:128], rhs=V[:, hs(h)], start=False, stop=True), reads=[M2s, V], writes=[bank[6]], inc=(h == 3))
        P.op("act", lambda e: e.activation(out=nr1[:], in_=bank[6][:, 0:256], func=AF.Copy, scale=-1.0), reads=[bank[6]], writes=[nr1])
        for h in range(4):
            P.op("pe", lambda e: e.matmul(bank[7][:, hs(h)], lhsT=TT[:, h, :], rhs=nr1[:, hs(h)], start=True, stop=True), reads=[TT, nr1], writes=[bank[7]], inc=(h == 3))
        P.op("dve", lambda e: e.tensor_copy(out=U[:], in_=bank[7][:, 0:256]), reads=[bank[7]], writes=[U])
        for h in range(4):
            P.op("pe", lambda e: e.matmul(bank[6][:, 256 + h*64:256 + (h+1)*64], lhsT=KR[:, h, 1, :], rhs=Sc[:, hs(h)], start=True, stop=False), reads=[KR, Sc], writes=[bank[6]], inc=False)
            P.op("pe", lambda e: e.matmul(bank[6][:, 256 + h*64:256 + (h+1)*64], lhsT=M1s[:, h, 128:256], rhs=U[:, hs(h)], start=False, stop=False), reads=[M1s, U], writes=[bank[6]], inc=False)
            P.op("pe", lambda e: e.matmul(bank[6][:, 256 + h*64:256 + (h+1)*64], lhsT=M2s[:, h, 128:256], rhs=V[:, hs(h)], start=False, stop=True), reads=[M2s, V], writes=[bank[6]], inc=(h == 3))
        yb = Y[c % 2]
        P.op("act", lambda e: e.copy(out=yb[:], in_=bank[6][:, 256:512]), reads=[bank[6]], writes=[yb])
        P.dma(yout[sl, :], yb[:], reads=[yb], writes=[(yout, c)], q="pool")
        for h in range(4):
            P.op("pe", lambda e: e.matmul(bank[7][0:64, 256 + h*64:256 + (h+1)*64], lhsT=diagG[:, hs(h)], rhs=Sc[:, hs(h)], start=True, stop=False), reads=[diagG, Sc], writes=[bank[7]], inc=False)
            P.op("pe", lambda e: e.matmul(bank[7][0:64, 256 + h*64:256 + (h+1)*64], lhsT=sc["bh"][:, hs(h)], rhs=U[:, hs(h)], start=False, stop=False), reads=[sc["bh"], U], writes=[bank[7]], inc=False)
            P.op("pe", lambda e: e.matmul(bank[7][0:64, 256 + h*64:256 + (h+1)*64], lhsT=sc["kh"][:, hs(h)], rhs=V[:, hs(h)], start=False, stop=True), reads=[sc["kh"], V], writes=[bank[7]], inc=(h == 3))
        P.op("dve", lambda e: e.tensor_copy(out=Sn[:], in_=bank[7][0:64, 256:512]), reads=[bank[7]], writes=[Sn])


def build_o2(S=8192):
    P = Prog()
    names = ["r", "lw", "k", "v", "kk", "b"]
    din = {n: P.dram(n, [S, 256]) for n in names}
    cshape = dict(tri_incl=[128, 128], tri_excl=[128, 128], tri_up=[128, 128], ones=[128, 128], maskM1=[128, 512], maskM2=[128, 512], negsl4=[128, 512],
                  identrep=[64, 256], ident4=[128, 512])
    cdram = {n_: P.dram(n_, shp) for n_, shp in cshape.items()}
    ident_d = P.dram("ident", [128, 128])
    yout = P.dram("y", [S, 256], kind="ExternalOutput")
    build_o2_body(P, din, yout, cdram, ident_d, S)
    P.finish([yout])
    return P


def _run(P, in_maps):
    res = run_bass_kernel_spmd(P.nc, in_maps, core_ids=list(range(8)))
    return res.results


def kernel(**inp):
    f32 = np.float32
    A = lambda a: np.ascontiguousarray(a, dtype=f32)
    x = A(inp["x"]).reshape(16384, 1024)
    p = A(inp["p"]).reshape(4, 16384, 256)
    ident = np.eye(128, dtype=f32)
    negtri, negones, masks = sb_consts()
    o2c = o2_consts()
    S = 8192
    h = x
    v_first = None
    sh = lambda a, c: A(a[c*2048:(c+1)*2048])
    cat = lambda r, n: np.concatenate([r[c][n] for c in range(8)], 0)
    for i in range(4):
        last = (i == 3)
        post_in = [{"h": sh(h, c), "p": sh(p[i], c), "ple_gate": A(inp["ple_gate"][i]), "ple_proj": A(inp["ple_proj"][i]),
                    "fgain": A(inp["final_gain"]).reshape(1, 1024), "ident": ident} for c in range(8)]
        if i % 2 == 0:
            e = i // 2
            g2 = A(inp["norm_gain"][i].reshape(8, 128).T)
            w_in = A(inp["even_w_in"][e])
            r = _run(build_e1(16), [{"h": sh(h, c), "gain": g2, "w_in": w_in, "ident": ident} for c in range(8)])
            z = cat(r, "z").reshape(2, S, 8, 4, 128)
            gnv = A(inp["ret_gn_gain"][e]).reshape(4, 1, 128)
            ims = []
            for c in range(8):
                b, hd = c // 4, c % 4
                d = dict(qa=A(z[b, :, 0, hd]), ka=A(z[b, :, 1, hd]), va=A(z[b, :, 2, hd]), ident=ident, gn=gnv[hd])
                d.update(ret_consts(hd, S))
                ims.append(d)
            r = _run(build_ret(S), ims)
            o_a = np.stack([np.stack([r[b*4+hd]["oa"] for hd in range(4)], 1) for b in range(2)], 0)
            ims = [dict(qb=A(z[c // 4, :, 4, c % 4]), kb=A(z[c // 4, :, 5, c % 4]), vb=A(z[c // 4, :, 6, c % 4]), ident=ident,
                        negtri=negtri, negones=negones, masks=masks) for c in range(8)]
            r = _run(build_sb(S), ims)
            o_b = np.stack([np.stack([r[b*4+hd]["obT"].T for hd in range(4)], 1) for b in range(2)], 0)
            o = A(np.concatenate([o_a.reshape(2, S, 512), o_b.reshape(2, S, 512)], -1).reshape(16384, 1024))
            g = A(np.concatenate([z[:, :, 3].reshape(2, S, 512), z[:, :, 7].reshape(2, S, 512)], -1).reshape(16384, 1024))
            for c in range(8):
                post_in[c].update(m=sh(o, c), g=sh(g, c), w_out=A(inp["even_w_out"][e]))
            mode = "gated"
        else:
            o = i // 2
            vres = v_first is not None
            ims = []
            for c in range(8):
                hprev = np.zeros((1, 1024), f32) if c % 4 == 0 else A(h[c*2048-1:c*2048])
                ims.append(o1_inputs(inp, o, i, sh(h, c), hprev, sh(v_first, c) if vres else None, vres))
            r = _run(build_o1(16, vres), ims)
            q = {n: cat(r, "o_" + n) for n in ["r", "lw", "k", "v", "kk", "b", "g"]}
            if v_first is None:
                v_first = q["v"]
            ims = []
            for c in range(8):
                b, hg = c // 4, c % 4
                d = {n: A(q[n].reshape(2, S, 1024)[b, :, hg*256:(hg+1)*256]) for n in ["r", "lw", "k", "v", "kk", "b"]}
                d.update(o2c)
                ims.append(d)
            r = _run(build_o2(S), ims)
            y = A(np.stack([np.concatenate([r[b*4+hg]["y"] for hg in range(4)], 1) for b in range(2)], 0).reshape(16384, 1024))
            for c in range(8):
                post_in[c].update(m=sh(y, c), g=sh(q["g"], c), r=sh(q["r"], c), k=sh(q["k"], c), v=sh(q["v"], c), w_out=A(inp["odd_w_out"][o]),
                                  lnx_g=A(inp["rwkv_lnx_gain"][o]).reshape(1, 1024), lnx_b=A(inp["rwkv_lnx_bias"][o]).reshape(1, 1024),
                                  r_k=A(inp["rwkv_r_k"][o]).reshape(1, 1024))
            mode = "rwkv"
        r = _run(build_post(16, final=last, mode=mode), post_in)
        h = cat(r, "out")
    return h.reshape(2, S, 1024).astype(f32)
```
